# Optimizing a Trainium2 kernel written in Bass

```python
import math
import jax
import jax.numpy as jnp
from jax import lax
import numpy as np

D_MODEL = 1024
BATCH = 4
SEQ = 8192
DEPTH = 2

CTX_LEN = 256
GRID_W = 64
CHUNK = 128
EPS = 1e-6

SSD_INNER = 2 * D_MODEL
SSD_HEAD_DIM = 64
SSD_HEADS = SSD_INNER // SSD_HEAD_DIM
SSD_GROUPS = 4
SSD_STATE = 128
SSD_CONV = 3
XBC_DIM = SSD_INNER + 2 * SSD_GROUPS * SSD_STATE

RET_HEADS = 8
RET_QK_DIM = D_MODEL // RET_HEADS
RET_V_DIM = 2 * RET_QK_DIM
RET_QK_WIDTH = RET_HEADS * RET_QK_DIM
RET_V_WIDTH = RET_HEADS * RET_V_DIM
ROPE_BASE = 10000.0

N_EXPERTS = 16
EXPERT_FF = 2 * D_MODEL
CAPACITY_FACTOR = 2

PROJ_SIZES = (SSD_INNER, XBC_DIM, SSD_HEADS, RET_QK_WIDTH, RET_QK_WIDTH, RET_V_WIDTH, RET_V_WIDTH, D_MODEL, D_MODEL)
PROJ_DIM = SSD_INNER + XBC_DIM + SSD_HEADS + 2 * RET_QK_WIDTH + 2 * RET_V_WIDTH + 2 * D_MODEL

kernel_name = 'hybrid_ssd_retention_ecmoe_dit'

F32 = jnp.float32


def rmsnorm(x, w):
    xf = x.astype(F32)
    y = xf * lax.rsqrt(jnp.mean(xf * xf, axis=-1, keepdims=True) + EPS)
    return (y * w.astype(F32)).astype(x.dtype)


def modulate(h, shift, scale):
    return h * (1 + scale) + shift


def split_cols(p, sizes):
    out, start = [], 0
    for s in sizes:
        out.append(p[..., start:start + s])
        start += s
    return out


def centred_dwconv(x, w, b):
    y = lax.conv_general_dilated(
        x, w[:, None, :].astype(x.dtype), window_strides=(1,),
        padding=((SSD_CONV // 2, SSD_CONV // 2),),
        dimension_numbers=('NWC', 'WIO', 'NWC'), feature_group_count=x.shape[-1])
    return y + b.astype(x.dtype)


def rope_2d(t):
    L = t.shape[1]
    rows = L // GRID_W
    r, col = jnp.meshgrid(jnp.arange(rows), jnp.arange(GRID_W), indexing='ij')
    n_freq = RET_QK_DIM // 4
    inv = ROPE_BASE ** (-jnp.arange(n_freq, dtype=F32) / n_freq)
    ang = jnp.concatenate([r.reshape(-1, 1).astype(F32) * inv, col.reshape(-1, 1).astype(F32) * inv], axis=-1)
    cos = jnp.cos(ang)[None, :, None, :]
    sin = jnp.sin(ang)[None, :, None, :]
    tf = t.astype(F32)
    t1, t2 = tf[..., :RET_QK_DIM // 2], tf[..., RET_QK_DIM // 2:]
    return jnp.concatenate([t1 * cos - t2 * sin, t1 * sin + t2 * cos], axis=-1).astype(t.dtype)


def chunked_scan(q, k, v, log_a, s0):
    bsz, L, G, N = q.shape
    Hg, P = v.shape[3], v.shape[4]
    nc = L // CHUNK

    def to_chunks(t):
        return jnp.moveaxis(t.astype(F32).reshape((bsz, nc, CHUNK) + t.shape[2:]), 1, 0)

    tril = jnp.tril(jnp.ones((CHUNK, CHUNK), dtype=bool))[None, :, :, None, None]

    def step(h, inp):
        qc, kc, vc, ac = inp
        cs = jnp.cumsum(ac, axis=1)
        decay = jnp.exp(jnp.where(tril, cs[:, :, None] - cs[:, None, :], -jnp.inf))
        scores = jnp.einsum('bign,bjgn->bijg', qc, kc)
        y = jnp.einsum('bijgh,bjghp->bighp', scores[..., None] * decay, vc)
        y = y + jnp.exp(cs)[..., None] * jnp.einsum('bign,bghnp->bighp', qc, h)
        last = cs[:, -1]
        kv = jnp.einsum('bjgn,bjghp->bghnp', kc, vc * jnp.exp(last[:, None] - cs)[..., None])
        h = jnp.exp(last)[..., None, None] * h + kv
        return h, y

    h_last, ys = lax.scan(step, s0.astype(F32), (to_chunks(q), to_chunks(k), to_chunks(v), to_chunks(log_a)))
    y = jnp.moveaxis(ys, 0, 1).reshape((bsz, L, G, Hg, P))
    return y, h_last


def directional_scan(qc, kc, vc, ac, ql, kl, vl, al, reverse):
    if reverse:
        qc, kc, vc, ac, ql, kl, vl, al = [jnp.flip(t, axis=1) for t in (qc, kc, vc, ac, ql, kl, vl, al)]
    bsz, G, Hg, P = vc.shape[0], vc.shape[2], vc.shape[3], vc.shape[4]
    N = qc.shape[-1]
    s0 = jnp.zeros((bsz, G, Hg, N, P), F32)
    yc, s_ctx = chunked_scan(qc, kc, vc, ac, s0)
    yl, _ = chunked_scan(ql, kl, vl, al, s_ctx)
    if reverse:
        yc, yl = jnp.flip(yc, axis=1), jnp.flip(yl, axis=1)
    return yc, yl


def ssd_branch(z, xbc, dt_raw, n_ctx, conv_w, conv_b, dt_bias, a_log, d_skip, norm_w):
    xbc = jax.nn.silu(jnp.concatenate([centred_dwconv(xbc[:, :n_ctx], conv_w, conv_b),
                                       centred_dwconv(xbc[:, n_ctx:], conv_w, conv_b)], axis=1))
    bsz, T = xbc.shape[:2]
    hg = SSD_HEADS // SSD_GROUPS
    xs, bm, cm = split_cols(xbc, (SSD_INNER, SSD_GROUPS * SSD_STATE, SSD_GROUPS * SSD_STATE))
    xs = xs.reshape(bsz, T, SSD_HEADS, SSD_HEAD_DIM).astype(F32)
    bm = bm.reshape(bsz, T, SSD_GROUPS, SSD_STATE)
    cm = cm.reshape(bsz, T, SSD_GROUPS, SSD_STATE)
    y = d_skip.astype(F32)[:, None] * xs
    for d in range(2):
        dt = jax.nn.softplus(dt_raw.astype(F32) + dt_bias[d].astype(F32))
        la = (dt * -jnp.exp(a_log[d].astype(F32))).reshape(bsz, T, SSD_GROUPS, hg)
        v = (xs * dt[..., None]).reshape(bsz, T, SSD_GROUPS, hg, SSD_HEAD_DIM)
        yc, yl = directional_scan(cm[:, :n_ctx], bm[:, :n_ctx], v[:, :n_ctx], la[:, :n_ctx],
                                  cm[:, n_ctx:], bm[:, n_ctx:], v[:, n_ctx:], la[:, n_ctx:], reverse=(d == 1))
        y = y + jnp.concatenate([yc, yl], axis=1).reshape(bsz, T, SSD_HEADS, SSD_HEAD_DIM)
    yg = (y.reshape(bsz, T, SSD_INNER) * jax.nn.silu(z.astype(F32))).reshape(bsz, T, SSD_GROUPS, SSD_INNER // SSD_GROUPS)
    yg = yg * lax.rsqrt(jnp.mean(yg * yg, axis=-1, keepdims=True) + EPS)
    return (yg.reshape(bsz, T, SSD_INNER) * norm_w.astype(F32)).astype(z.dtype)


def ret_branch(q, k, v, g, n_ctx, decay_logit, gn_w):
    bsz, T = q.shape[:2]
    q = q.reshape(bsz, T, RET_HEADS, RET_QK_DIM)
    k = k.reshape(bsz, T, RET_HEADS, RET_QK_DIM) * (RET_QK_DIM ** -0.5)
    v = v.reshape(bsz, T, RET_HEADS, 1, RET_V_DIM)
    qc, kc = q[:, :n_ctx], k[:, :n_ctx]
    ql, kl = rope_2d(q[:, n_ctx:]), rope_2d(k[:, n_ctx:])
    ys = []
    for d in range(2):
        la = jax.nn.log_sigmoid(decay_logit[d].astype(F32))[:, None]
        lac = jnp.broadcast_to(la, (bsz, n_ctx, RET_HEADS, 1))
        lal = jnp.broadcast_to(la, (bsz, T - n_ctx, RET_HEADS, 1))
        yc, yl = directional_scan(qc, kc, v[:, :n_ctx], lac, ql, kl, v[:, n_ctx:], lal, reverse=(d == 1))
        ys.append(jnp.concatenate([yc, yl], axis=1))
    y = (ys[0] + ys[1]).reshape(bsz, T, RET_HEADS, RET_V_DIM)
    mu = jnp.mean(y, axis=-1, keepdims=True)
    yc0 = y - mu
    y = yc0 * lax.rsqrt(jnp.mean(yc0 * yc0, axis=-1, keepdims=True) + EPS)
    y = y.reshape(bsz, T, RET_V_WIDTH) * gn_w.astype(F32)
    return (jax.nn.silu(g.astype(F32)) * y).astype(g.dtype)


def hybrid_mixer(h_c, h_l, w_in, conv_w, conv_b, dt_bias, a_log, d_skip, ssd_norm_w, ret_decay, ret_gn_w,
                 w_ssd_o, w_ret_o, w_o):
    n_ctx = h_c.shape[1]
    p = jnp.concatenate([h_c, h_l], axis=1) @ w_in
    z, xbc, dt_raw, q, k, v, g, gate_s, gate_r = split_cols(p, PROJ_SIZES)
    y_s = ssd_branch(z, xbc, dt_raw, n_ctx, conv_w, conv_b, dt_bias, a_log, d_skip, ssd_norm_w) @ w_ssd_o
    y_r = ret_branch(q, k, v, g, n_ctx, ret_decay, ret_gn_w) @ w_ret_o
    m = jax.nn.sigmoid(gate_s) * y_s + jax.nn.sigmoid(gate_r) * y_r
    out = m @ w_o
    return out[:, :n_ctx], out[:, n_ctx:]


def expert_choice_ffn(h, w_router, w_gate, w_up, w_down):
    bsz, L, _ = h.shape
    cap = CAPACITY_FACTOR * L // N_EXPERTS
    aff = jax.nn.softmax((h @ w_router).astype(F32), axis=-1)
    gsel, idx = lax.top_k(jnp.swapaxes(aff, 1, 2), cap)
    bidx = jnp.arange(bsz)[:, None, None]
    xin = h[bidx, idx]
    hid = jax.nn.silu(jnp.einsum('becd,edf->becf', xin, w_gate)) * jnp.einsum('becd,edf->becf', xin, w_up)
    out = jnp.einsum('becf,efd->becd', hid, w_down) * gsel[..., None].astype(h.dtype)
    return jnp.zeros_like(h).at[bidx, idx].add(out)


def setup_inputs(seed: int = 0) -> dict:
    key = jax.random.key(seed)
    ks = jax.random.split(key, 26)
    D = D_MODEL

    def nrm(k, shape, scale):
        return jax.random.normal(k, shape, F32) * scale

    x = nrm(ks[0], (BATCH, SEQ, D), 1.0)
    c = nrm(ks[1], (BATCH, D), 1.0)
    ctx = nrm(ks[2], (BATCH, CTX_LEN, D), 1.0)
    c_ctx = nrm(ks[3], (D,), 1.0)
    w_mod = nrm(ks[4], (DEPTH, D, 6 * D), D ** -0.5)
    b_mod = nrm(ks[5], (DEPTH, 6 * D), 0.01)
    norm_mix_w = 1.0 + nrm(ks[6], (DEPTH, D), 0.01)
    w_in = nrm(ks[7], (DEPTH, D, PROJ_DIM), D ** -0.5)
    conv_w = nrm(ks[8], (DEPTH, SSD_CONV, XBC_DIM), SSD_CONV ** -0.5)
    conv_b = nrm(ks[9], (DEPTH, XBC_DIM), 0.01)
    dt0 = jnp.exp(jax.random.uniform(ks[10], (DEPTH, 2, SSD_HEADS), F32, math.log(1e-3), math.log(1e-1)))
    ssd_dt_bias = dt0 + jnp.log(-jnp.expm1(-dt0))
    ssd_a_log = jnp.log(jax.random.uniform(ks[11], (DEPTH, 2, SSD_HEADS), F32, 1.0, 16.0))
    ssd_d = 1.0 + nrm(ks[12], (DEPTH, SSD_HEADS), 0.1)
    ssd_norm_w = 1.0 + nrm(ks[13], (DEPTH, SSD_INNER), 0.01)
    gamma0 = 1.0 - 2.0 ** (-5.0 - jnp.arange(RET_HEADS, dtype=F32))
    ret_decay = jnp.log(gamma0 / (1.0 - gamma0)) + nrm(ks[14], (DEPTH, 2, RET_HEADS), 0.01)
    ret_gn_w = 1.0 + nrm(ks[15], (DEPTH, RET_V_WIDTH), 0.01)
    w_ssd_o = nrm(ks[16], (DEPTH, SSD_INNER, D), SSD_INNER ** -0.5)
    w_ret_o = nrm(ks[17], (DEPTH, RET_V_WIDTH, D), RET_V_WIDTH ** -0.5)
    w_o = nrm(ks[18], (DEPTH, D, D), D ** -0.5)
    norm_ffn_w = 1.0 + nrm(ks[19], (DEPTH, D), 0.01)
    w_router = nrm(ks[20], (DEPTH, D, N_EXPERTS), D ** -0.5)
    w_gate = nrm(ks[21], (DEPTH, N_EXPERTS, D, EXPERT_FF), D ** -0.5)
    w_up = nrm(ks[22], (DEPTH, N_EXPERTS, D, EXPERT_FF), D ** -0.5)
    w_down = nrm(ks[23], (DEPTH, N_EXPERTS, EXPERT_FF, D), EXPERT_FF ** -0.5)
    final_norm_w = 1.0 + nrm(ks[24], (D,), 0.01)
    return {'x': x, 'c': c, 'ctx': ctx, 'c_ctx': c_ctx, 'w_mod': w_mod, 'b_mod': b_mod,
            'norm_mix_w': norm_mix_w, 'w_in': w_in, 'conv_w': conv_w, 'conv_b': conv_b,
            'ssd_dt_bias': ssd_dt_bias, 'ssd_a_log': ssd_a_log, 'ssd_d': ssd_d, 'ssd_norm_w': ssd_norm_w,
            'ret_decay': ret_decay, 'ret_gn_w': ret_gn_w, 'w_ssd_o': w_ssd_o, 'w_ret_o': w_ret_o, 'w_o': w_o,
            'norm_ffn_w': norm_ffn_w, 'w_router': w_router, 'w_gate': w_gate, 'w_up': w_up, 'w_down': w_down,
            'final_norm_w': final_norm_w}


def reference(x, c, ctx, c_ctx, w_mod, b_mod, norm_mix_w, w_in, conv_w, conv_b, ssd_dt_bias, ssd_a_log, ssd_d,
              ssd_norm_w, ret_decay, ret_gn_w, w_ssd_o, w_ret_o, w_o, norm_ffn_w, w_router, w_gate, w_up, w_down,
              final_norm_w):
    bsz = x.shape[0]
    h_ctx = ctx
    for l in range(DEPTH):
        last = l == DEPTH - 1
        mod = (jax.nn.silu(c) @ w_mod[l] + b_mod[l]).reshape(bsz, 6, 1, D_MODEL)
        mod_c = (jax.nn.silu(c_ctx) @ w_mod[l] + b_mod[l]).reshape(6, 1, 1, D_MODEL)
        hl = modulate(rmsnorm(x, norm_mix_w[l]), mod[:, 0], mod[:, 1])
        hc = modulate(rmsnorm(h_ctx, norm_mix_w[l]), mod_c[0], mod_c[1])
        mix_c, mix_l = hybrid_mixer(hc, hl, w_in[l], conv_w[l], conv_b[l], ssd_dt_bias[l], ssd_a_log[l], ssd_d[l],
                                    ssd_norm_w[l], ret_decay[l], ret_gn_w[l], w_ssd_o[l], w_ret_o[l], w_o[l])
        x = x + mod[:, 2] * mix_l
        hl2 = modulate(rmsnorm(x, norm_ffn_w[l]), mod[:, 3], mod[:, 4])
        x = x + mod[:, 5] * expert_choice_ffn(hl2, w_router[l], w_gate[l], w_up[l], w_down[l])
        if not last:
            h_ctx = h_ctx + mod_c[2] * mix_c
            hc2 = modulate(rmsnorm(h_ctx, norm_ffn_w[l]), mod_c[3], mod_c[4])
            h_ctx = h_ctx + mod_c[5] * expert_choice_ffn(hc2, w_router[l], w_gate[l], w_up[l], w_down[l])
    return rmsnorm(x, final_norm_w)
```

```python
import contextlib
import numpy as np
import concourse.bass as bass
import concourse.mybir as mybir
from concourse.bass_utils import run_bass_kernel_spmd

F32 = mybir.dt.float32
BF16 = mybir.dt.bfloat16
I32 = mybir.dt.int32
I16 = mybir.dt.int16
AF = mybir.ActivationFunctionType
ALU = mybir.AluOpType
AX = mybir.AxisListType


class KB:
    N_DMA_SEMS = 40

    def __init__(self, nc):
        self.nc = nc
        self.stack = contextlib.ExitStack()
        self.stack0 = self.stack
        self.PE, self.ACT, self.DVE, self.POOL, self.SP = nc.tensor, nc.scalar, nc.vector, nc.gpsimd, nc.sync
        self.engs = [self.PE, self.ACT, self.DVE, self.POOL, self.SP]
        self.esem = {}
        self.ecnt = {}
        for i, e in enumerate(self.engs):
            self.esem[id(e)] = self.stack.enter_context(nc.semaphore(f"es{i}"))
            self.ecnt[id(e)] = 0
        self.dsems = [self.stack.enter_context(nc.semaphore(f"ds{i}")) for i in range(self.N_DMA_SEMS)]
        self.dcnt = [0] * self.N_DMA_SEMS
        self.dnext = 0
        self.seen = {id(e): {} for e in self.engs}
        self.semobj = {}
        self.lastw = {}
        self.reads = {}
        self.out_events = []
        self.n_inst = 0
        self.n_wait = 0

    def _uname(self, name):
        self.uid = getattr(self, "uid", 0) + 1
        return f"{name}_u{self.uid}"

    def sb(self, name, shape, dtype):
        return self.stack.enter_context(self.nc.sbuf_tensor(self._uname("s_" + name), list(shape), dtype))

    def ps(self, name, shape, dtype=F32):
        return self.stack.enter_context(self.nc.psum_tensor(self._uname("p_" + name), list(shape), dtype))

    @staticmethod
    def _key(b):
        return b if isinstance(b, (tuple, str, int)) else id(b)

    def _wait(self, eng, ev):
        sem, val = ev
        seen = self.seen[id(eng)]
        if seen.get(id(sem), 0) >= val:
            return
        eng.wait_ge(sem, val)
        self.n_wait += 1
        seen[id(sem)] = val

    def _deps(self, eng, r, w, same_engine_ok=False):
        evs = []
        for b in list(r) + list(w):
            ev = self.lastw.get(self._key(b))
            if ev is not None:
                evs.append(ev)
        for b in w:
            evs.extend(self.reads.get(self._key(b), []))
        mysem = self.esem[id(eng)]
        for ev in evs:
            if same_engine_ok and ev[0] is mysem:
                continue
            self._wait(eng, ev)

    def _record(self, ev, r, w):
        for b in r:
            self.reads.setdefault(self._key(b), []).append(ev)
        for b in w:
            k = self._key(b)
            self.lastw[k] = ev
            self.reads[k] = []

    def op(self, eng, fn, r=(), w=()):
        self._deps(eng, r, w, same_engine_ok=(eng is self.PE))
        inst = fn(eng)
        sem = self.esem[id(eng)]
        self.ecnt[id(eng)] += 1
        inst.then_inc(sem, 1)
        ev = (sem, self.ecnt[id(eng)])
        self._record(ev, r, w)
        self.n_inst += 1
        return ev

    def dma(self, q, out, in_, r=(), w=(), is_output=False, **kw):
        self._deps(q, r, w)
        import os
        if q is self.POOL and os.environ.get("KB_UNIQUE_POOL_SEMS"):
            self.dsems.append(self.stack0.enter_context(self.nc.semaphore(f"dsx{len(self.dsems)}")))
            self.dcnt.append(0)
            slot = len(self.dsems) - 1
        else:
            slot = self.dnext
            self.dnext = (self.dnext + 1) % self.N_DMA_SEMS
        sem = self.dsems[slot]
        if self.dcnt[slot] > 0:
            self._wait(q, (sem, self.dcnt[slot]))
        inst = out(q) if callable(out) else q.dma_start(out=out, in_=in_, **kw)
        self.dcnt[slot] += 16
        inst.then_inc(sem, 16)
        ev = (sem, self.dcnt[slot])
        self._record(ev, r, w)
        if is_output:
            self.out_events.append(ev)
        self.n_inst += 1
        return ev

    def _finish_dma(self, q, inst, r, w):
        slot = self.dnext
        self.dnext = (self.dnext + 1) % self.N_DMA_SEMS
        sem = self.dsems[slot]
        self.dcnt[slot] += 16
        inst.then_inc(sem, 16)
        ev = (sem, self.dcnt[slot])
        self._record(ev, r, w)
        self.n_inst += 1
        return ev

    def bound_reg(self, val):
        regs = self.__dict__.setdefault("_bregs", {})
        if val not in regs:
            regs[val] = self.nc.gpsimd.to_reg(val)
        return regs[val]

    def finish(self):
        for ev in self.out_events:
            self._wait(self.SP, ev)
        for slot in range(len(self.dsems)):
            if self.dcnt[slot] > 0:
                self._wait(self.SP, (self.dsems[slot], self.dcnt[slot]))
        for e in self.engs:
            if e is not self.SP and self.ecnt[id(e)] > 0:
                self._wait(self.SP, (self.esem[id(e)], self.ecnt[id(e)]))
        self.stack.close()


    def barrier(self):
        evs = []
        for e in self.engs:
            if self.ecnt[id(e)] > 0:
                evs.append((self.esem[id(e)], self.ecnt[id(e)]))
        for slot in range(len(self.dsems)):
            if self.dcnt[slot] > 0:
                evs.append((self.dsems[slot], self.dcnt[slot]))
        for e in self.engs:
            for ev in evs:
                self._wait(e, ev)
        self.lastw.clear()
        self.reads.clear()

    @contextlib.contextmanager
    def scope(self):
        outer = self.stack
        self.stack = contextlib.ExitStack()
        try:
            yield
        finally:
            self.barrier()
            self.stack.close()
            self.stack = outer

    def mm(self, out, lhsT, rhs, start=True, stop=True, r=(), w=()):
        return self.op(self.PE, lambda e: e.matmul(out, lhsT=lhsT, rhs=rhs, start=start, stop=stop), r=r, w=w)

    def act(self, out, in_, func, r=(), w=(), **kw):
        return self.op(self.ACT, lambda e: e.activation(out=out, in_=in_, func=func, **kw), r=r, w=w)

    def tt(self, eng, out, in0, in1, op, r=(), w=()):
        return self.op(eng, lambda e: e.tensor_tensor(out=out, in0=in0, in1=in1, op=op), r=r, w=w)

    def ts(self, eng, out, in0, s1, op0, s2=None, op1=None, r=(), w=(), **kw):
        if op1 is None:
            return self.op(eng, lambda e: e.tensor_scalar(out=out, in0=in0, scalar1=s1, scalar2=None, op0=op0, **kw), r=r, w=w)
        return self.op(eng, lambda e: e.tensor_scalar(out=out, in0=in0, scalar1=s1, scalar2=s2, op0=op0, op1=op1, **kw), r=r, w=w)

    def stt(self, eng, out, in0, scalar, in1, op0, op1, r=(), w=()):
        return self.op(eng, lambda e: e.scalar_tensor_tensor(out=out, in0=in0, scalar=scalar, in1=in1, op0=op0, op1=op1), r=r, w=w)

    def copy(self, eng, out, in_, r=(), w=()):
        if eng is self.ACT:
            return self.op(eng, lambda e: e.copy(out=out, in_=in_), r=r, w=w)
        return self.op(eng, lambda e: e.tensor_copy(out=out, in_=in_), r=r, w=w)

    def memset(self, eng, ap, val, w=()):
        return self.op(eng, lambda e: e.memset(ap, val), w=w)

    def transpose(self, out, in_, ident, r=(), w=()):
        return self.op(self.PE, lambda e: e.transpose(out, in_, ident), r=r, w=w)


class Ring:
    def __init__(self, K, name, shape, dtype, n, psum=False):
        self.bufs = [(K.ps if psum else K.sb)(f"{name}{i}", shape, dtype) for i in range(n)]
        self.i = 0

    def next(self):
        b = self.bufs[self.i % len(self.bufs)]
        self.i += 1
        return b


EPS = 1e-6
NEG = -60000.0
N_TM = 9 * 512
N_FM = 28 * 128
C_Z, C_Q, C_K, C_V, C_G, C_DT = 0, 1024, 1536, 2048, 3072, 4096


class Dims:
    def __init__(self, ntc=2, ntl=64):
        self.ntc, self.ntl = ntc, ntl
        self.nt = ntc + ntl
        self.T = 128 * self.nt
        self.TC = 128 * ntc
        self.TL = 128 * ntl
        self.blocks = [(0, self.TC, 1)] + [(self.TC + i * 512, 512, 0) for i in range(self.TL // 512)]


def make_consts(K):
    c = {}
    def tri(name, pattern, cm, op, base_val, fill):
        t = K.sb(name, [128, 128], F32)
        K.memset(K.POOL, t[:], base_val, w=[t])
        K.op(K.POOL, lambda e: e.affine_select(out=t[:], in_=t[:], pattern=pattern, compare_op=op, fill=fill,
                                               base=0, channel_multiplier=cm), r=[t], w=[t])
        return t
    c["ident"] = tri("ident", [[-1, 128]], 1, ALU.is_equal, 1.0, 0.0)
    c["U0"] = tri("Ufwd", [[1, 128]], -1, ALU.is_ge, 1.0, 0.0)
    c["U1"] = tri("Urev", [[-1, 128]], 1, ALU.is_ge, 1.0, 0.0)
    c["M0"] = tri("Mfwd", [[1, 128]], -1, ALU.is_ge, 0.0, NEG)
    c["M1"] = tri("Mrev", [[-1, 128]], 1, ALU.is_ge, 0.0, NEG)
    ones = K.sb("ones", [128, 128], F32)
    K.memset(K.POOL, ones[:], 1.0, w=[ones])
    c["ones"] = ones
    idb = K.sb("identb", [128, 128], BF16)
    K.copy(K.DVE, idb[:], c["ident"][:], r=[c["ident"]], w=[idb])
    c["identb"] = idb
    return c


def rsqrt_mean(K, out, in_, n, r, w):
    K.act(out, in_, AF.Ln, r=r, w=w, scale=1.0 / n, bias=EPS)
    K.act(out, out, AF.Exp, r=w, w=w, scale=-0.5)


def build_mod(K, nc, D, I, l, modT, cst):
    with K.scope():
        condT = K.sb("condT", [128, 8, 2], F32)
        sc = K.sb("siluc", [128, 8, 2], F32)
        bm = K.sb("bmT", [128, 48], F32)
        K.dma(K.SP, condT[:], I["condT"], w=[condT])
        K.dma(K.SP, bm[:], I[f"b_modT{l}"], w=[bm])
        K.act(sc[:], condT[:], AF.Silu, r=[condT], w=[sc])
        wring = Ring(K, "wm", [128, 8, 768], F32, 2)
        pring = Ring(K, "pmod", [128, 512], F32, 2, psum=True)
        wv = I[f"w_mod{l}"].rearrange("(k p) c -> p k c", p=128)
        for jb in range(8):
            wm = wring.next()
            K.dma(K.SP if jb % 2 == 0 else K.ACT, wm[:], wv[:, :, jb * 768:(jb + 1) * 768], w=[wm])
            for j6 in range(6):
                j = jb * 6 + j6
                ps = pring.next()
                for k in range(8):
                    K.mm(ps[:, 0:2], wm[:, k, j6 * 128:(j6 + 1) * 128], sc[:, k, :], start=(k == 0), stop=(k == 7),
                         r=[wm, sc], w=[ps])
                K.ts(K.DVE, modT[:, j, :], ps[:, 0:2], bm[:, j:j + 1], ALU.add, r=[ps, bm], w=[modT])


def build_norm_mod(K, D, src_aps, normw, modT, row_shift, row_scale, hlT, cst, xsum_out=None):
    with K.scope():
        A = K.sb("A", [128, 8, 2], F32)
        K.ts(K.DVE, A[:], modT[:, row_scale * 8:(row_scale + 1) * 8, :], 1.0, ALU.add, r=[modT], w=[A])
        K.tt(K.DVE, A[:], A[:], normw[:].unsqueeze(2).to_broadcast([128, 8, 2]), ALU.mult, r=[A, normw], w=[A])
        xring = Ring(K, "xb", [128, 8, 512], F32, 2)
        x2ring = Ring(K, "xb2", [128, 8, 512], F32, 1) if len(src_aps) > 1 else None
        sq = K.sb("sq", [128, 8, 512], F32) if x2ring is None else x2ring.bufs[0]
        rstd = K.sb("rstd", [128, 512], F32)
        tring = Ring(K, "tmpn", [128, 512], F32, 2)
        pring = Ring(K, "pn", [128, 512], F32, 2, psum=True)
        for bi, (s0, n, r) in enumerate(D.blocks):
            xb = xring.next()
            K.dma(K.SP, xb[:, :, :n], src_aps[0][:, s0:s0 + n].rearrange("(k p) t -> p k t", p=128), w=[xb])
            for extra in src_aps[1:]:
                x2 = x2ring.next()
                K.dma(K.ACT, x2[:, :, :n], extra[:, s0:s0 + n].rearrange("(k p) t -> p k t", p=128), w=[x2])
                K.tt(K.POOL, xb[:, :, :n], xb[:, :, :n], x2[:, :, :n], ALU.add, r=[xb, x2], w=[xb])
            if xsum_out is not None:
                K.dma(K.SP, xsum_out[:, s0:s0 + n].rearrange("(k p) t -> p k t", p=128), xb[:, :, :n], r=[xb])
            K.act(sq[:, :, :n], xb[:, :, :n], AF.Square, r=[xb], w=[sq])
            ps = pring.next()
            for k in range(8):
                K.mm(ps[:, :n], cst["ones"][:], sq[:, k, :n], start=(k == 0), stop=(k == 7), r=[sq], w=[ps])
            rsqrt_mean(K, rstd[:, :n], ps[:, :n], 1024, r=[ps], w=[rstd])
            for k in range(8):
                tmp = tring.next()
                K.tt(K.DVE, tmp[:, :n], xb[:, k, :n], rstd[:, :n], ALU.mult, r=[xb, rstd], w=[tmp])
                K.act(hlT[:, k, s0:s0 + n], tmp[:, :n], AF.Identity, r=[tmp, A, modT], w=[(id(hlT), bi)],
                      scale=A[:, k, r:r + 1], bias=modT[:, row_shift * 8 + k, r:r + 1])


def build_inproj(K, D, I, l, hlT, S, cst):
    W = I[f"w_core{l}"]
    with K.scope():
        wring = Ring(K, "wtm", [128, 8, 512], BF16, 2)
        pring = Ring(K, "pip", [128, 512], F32, 4, psum=True)
        sring = Ring(K, "stg", [128, 512], F32, 4)
        cnt = 0
        for cb in range(9):
            wb = wring.next()
            K.dma(K.POOL, wb[:], W[:, cb * 512:(cb + 1) * 512].rearrange("(k p) c -> p k c", p=128), w=[wb])
            for tt_ in range(D.nt):
                ps = pring.next()
                for k in range(8):
                    K.mm(ps[:], hlT[:, k, tt_ * 128:(tt_ + 1) * 128], wb[:, k, :], start=(k == 0), stop=(k == 7),
                         r=[wb], w=[ps])
                st = sring.next()
                if cb == 3:
                    K.ts(K.DVE, st[:], ps[:], float(128 ** -0.5), ALU.mult, r=[ps], w=[st])
                elif cnt % 2 == 0:
                    K.copy(K.ACT, st[:], ps[:], r=[ps], w=[st])
                else:
                    K.copy(K.DVE, st[:], ps[:], r=[ps], w=[st])
                cnt += 1
                K.dma(K.SP, S["P_tm"][tt_ * 128:(tt_ + 1) * 128, cb * 512:(cb + 1) * 512], st[:], r=[st])
        wring2 = Ring(K, "wfm", [128, 8, 128], BF16, 2)
        for c in range(28):
            wb = wring2.next()
            K.dma(K.POOL, wb[:], W[:, N_TM + c * 128:N_TM + (c + 1) * 128].rearrange("(k p) c -> p k c", p=128), w=[wb])
            for (s0, n, r) in D.blocks:
                ps = pring.next()
                for k in range(8):
                    K.mm(ps[:, :n], wb[:, k, :], hlT[:, k, s0:s0 + n], start=(k == 0), stop=(k == 7), r=[wb], w=[ps])
                st = sring.next()
                if c >= 12:
                    K.act(st[:, :n], ps[:, :n], AF.Sigmoid, r=[ps], w=[st])
                elif cnt % 2 == 0:
                    K.copy(K.ACT, st[:, :n], ps[:, :n], r=[ps], w=[st])
                else:
                    K.copy(K.DVE, st[:, :n], ps[:, :n], r=[ps], w=[st])
                cnt += 1
                K.dma(K.SP, S["PT_fm"][c * 128:(c + 1) * 128, s0:s0 + n], st[:, :n], r=[st])


def build_conv(K, D, I, l, S, cst):
    with K.scope():
        cw = K.sb("cw", [128, 12, 3], F32)
        cb = K.sb("cb", [128, 12], F32)
        K.dma(K.SP, cw[:], I[f"cw{l}"], w=[cw])
        K.dma(K.SP, cb[:], I[f"cbias{l}"], w=[cb])
        cin_ring = Ring(K, "cin", [128, 514], F32, 3)
        acc_ring = Ring(K, "cacc", [128, 512], F32, 2)
        co_ring = Ring(K, "cout", [128, 512], F32, 3)
        cob_ring = Ring(K, "coutb", [128, 512], BF16, 3)
        xs_stage = Ring(K, "xsst", [128, 4, 1024], F32, 2)
        bm_stage = Ring(K, "bmst", [128, 4, 256], BF16, 2)
        pring = Ring(K, "pcv", [128, 512], F32, 4, psum=True)
        for (s0, n, r) in D.blocks:
            first = (s0 == 0) or (s0 == D.TC)
            last = (s0 + n == D.TC) or (s0 + n == D.T)
            nt4 = n // 128
            xst = xs_stage.next()
            bst = bm_stage.next()
            for c in range(12):
                cin = cin_ring.next()
                lo = s0 - (0 if first else 1)
                hi = s0 + n + (0 if last else 1)
                if first:
                    K.memset(K.POOL, cin[:, 0:1], 0.0, w=[cin])
                if last:
                    K.memset(K.POOL, cin[:, n + 1:n + 2], 0.0, w=[cin])
                K.dma(K.SP if c % 2 == 0 else K.ACT, cin[:, (1 if first else 0):(1 if first else 0) + hi - lo],
                      S["PT_fm"][c * 128:(c + 1) * 128, lo:hi], w=[cin])
                acc = acc_ring.next()
                K.ts(K.DVE, acc[:, :n], cin[:, 0:n], cw[:, c, 0:1], ALU.mult, r=[cin, cw], w=[acc])
                K.stt(K.DVE, acc[:, :n], cin[:, 1:n + 1], cw[:, c, 1:2], acc[:, :n], ALU.mult, ALU.add, r=[cin, acc], w=[acc])
                K.stt(K.DVE, acc[:, :n], cin[:, 2:n + 2], cw[:, c, 2:3], acc[:, :n], ALU.mult, ALU.add, r=[cin, acc], w=[acc])
                if c < 8:
                    co = co_ring.next()
                    K.act(co[:, :n], acc[:, :n], AF.Silu, r=[acc, cb], w=[co], bias=cb[:, c:c + 1])
                    ps = pring.next()
                    for j in range(nt4):
                        K.transpose(ps[:, j * 128:(j + 1) * 128], co[:, j * 128:(j + 1) * 128], cst["ident"][:], r=[co], w=[ps])
                    K.copy(K.ACT if c % 2 == 0 else K.DVE, xst[:, :nt4, c * 128:(c + 1) * 128],
                           ps[:, :n].rearrange("p (j q) -> p j q", q=128), r=[ps], w=[xst])
                else:
                    cob = cob_ring.next()
                    K.act(cob[:, :n], acc[:, :n], AF.Silu, r=[acc, cb], w=[cob], bias=cb[:, c:c + 1])
                    dst = S["BT"] if c < 10 else S["CT"]
                    cc = (c - 8) if c < 10 else (c - 10)
                    K.dma(K.SP, dst[cc * 128:(cc + 1) * 128, s0:s0 + n], cob[:, :n], r=[cob])
                    if c < 10:
                        ps = pring.next()
                        psb = ps[:].bitcast(BF16)
                        for j in range(nt4):
                            K.transpose(psb[:, j * 128:(j + 1) * 128], cob[:, j * 128:(j + 1) * 128], cst["identb"][:], r=[cob], w=[ps])
                        K.copy(K.DVE, bst[:, :nt4, cc * 128:(cc + 1) * 128],
                               psb[:, :n].rearrange("p (j q) -> p j q", q=128), r=[ps], w=[bst])
            K.dma(K.SP, S["XS_tm"][s0:s0 + n, :].rearrange("(j p) c -> p j c", p=128), xst[:, :nt4, :], r=[xst])
            K.dma(K.ACT, S["Bm"][s0:s0 + n, :].rearrange("(j p) c -> p j c", p=128), bst[:, :nt4, :], r=[bst])


def build_rope(K, D, I, S, cst):
    with K.scope():
        qk_ring = Ring(K, "qk", [128, 1024], F32, 2)
        cs_ring = Ring(K, "cossin", [128, 128], F32, 2)
        ro_ring = Ring(K, "ro", [128, 1024], F32, 2)
        t_ring = Ring(K, "rt", [128, 8, 64], F32, 2)
        rb_ring = Ring(K, "rob", [128, 1024], BF16, 2)
        tst = Ring(K, "rtst", [128, 8, 128], BF16, 2)
        pring = Ring(K, "prp", [128, 512], F32, 2, psum=True)
        for t in range(D.nt):
            qk = qk_ring.next()
            K.dma(K.SP, qk[:], S["P_tm"][t * 128:(t + 1) * 128, C_Q:C_Q + 1024], w=[qk])
            rb = rb_ring.next()
            if t >= D.ntc:
                tl = t - D.ntc
                cs = cs_ring.next()
                K.dma(K.ACT, cs[:, 0:64], I["cos"][tl * 128:(tl + 1) * 128, :], w=[cs])
                K.dma(K.ACT, cs[:, 64:128], I["sin"][tl * 128:(tl + 1) * 128, :], w=[cs])
                v = qk[:].rearrange("p (h two d) -> p h two d", two=2, d=64)
                t1, t2 = v[:, :, 0, :], v[:, :, 1, :]
                cosb = cs[:, 0:64].unsqueeze(1).to_broadcast([128, 8, 64])
                sinb = cs[:, 64:128].unsqueeze(1).to_broadcast([128, 8, 64])
                ro = ro_ring.next()
                rv = ro[:].rearrange("p (h two d) -> p h two d", two=2, d=64)
                ta = t_ring.next()
                K.tt(K.DVE, rv[:, :, 0, :], t1, cosb, ALU.mult, r=[qk, cs], w=[ro])
                K.tt(K.POOL, ta[:], t2, sinb, ALU.mult, r=[qk, cs], w=[ta])
                K.tt(K.DVE, rv[:, :, 0, :], rv[:, :, 0, :], ta[:], ALU.subtract, r=[ro, ta], w=[ro])
                tb = t_ring.next()
                K.tt(K.DVE, rv[:, :, 1, :], t1, sinb, ALU.mult, r=[qk, cs], w=[ro])
                K.tt(K.POOL, tb[:], t2, cosb, ALU.mult, r=[qk, cs], w=[tb])
                K.tt(K.DVE, rv[:, :, 1, :], rv[:, :, 1, :], tb[:], ALU.add, r=[ro, tb], w=[ro])
                K.copy(K.ACT, rb[:], ro[:], r=[ro], w=[rb])
            else:
                K.copy(K.ACT, rb[:], qk[:], r=[qk], w=[rb])
            K.dma(K.SP, S["Km_r"][t * 128:(t + 1) * 128, :], rb[:, 512:1024], r=[rb])
            st = tst.next()
            for half in range(2):
                ps = pring.next()
                psb = ps[:].bitcast(BF16)
                for j in range(4):
                    jj = half * 4 + j
                    K.transpose(psb[:, j * 128:(j + 1) * 128], rb[:, jj * 128:(jj + 1) * 128], cst["identb"][:], r=[rb], w=[ps])
                K.copy(K.DVE if half == 0 else K.ACT, st[:, half * 4:(half + 1) * 4, :],
                       psb[:, 0:512].rearrange("p (j q) -> p j q", q=128), r=[ps], w=[st])
            K.dma(K.SP, S["QT_r"][:, t * 128:(t + 1) * 128].rearrange("(j p) t -> p j t", p=128), st[:, 0:4, :], r=[st])
            K.dma(K.ACT, S["KT_r"][:, t * 128:(t + 1) * 128].rearrange("(j p) t -> p j t", p=128), st[:, 4:8, :], r=[st])


def build_scan(K, D, I, l, S, cst):
    with K.scope():
        def bload(name, ap, n):
            t = K.sb(name, [128, n], F32)
            K.dma(K.SP, t[:], ap.partition_broadcast(128), w=[t])
            return t
        dtb = bload("dtb", I[f"dtb{l}"], 32)
        negA = bload("negA", I[f"alog{l}"], 32)
        dsk = bload("dsk", I[f"dsk{l}"], 16)
        snw = bload("snw", I[f"snw{l}"], 1024)
        gnw = bload("gnw", I[f"gnw{l}"], 1024)
        laret = bload("laret", I[f"rdec{l}"], 8)
        K.act(negA[:], negA[:], AF.Exp, r=[negA], w=[negA])
        K.ts(K.DVE, negA[:], negA[:], -1.0, ALU.mult, r=[negA], w=[negA])
        K.act(laret[:], laret[:], AF.Exp, r=[laret], w=[laret], scale=-1.0)
        K.act(laret[:], laret[:], AF.Ln, r=[laret], w=[laret], bias=1.0)
        K.ts(K.DVE, laret[:], laret[:], -1.0, ALU.mult, r=[laret], w=[laret])

        qt_ring = Ring(K, "qt", [128, 128], BF16, 4)
        kt_ring = Ring(K, "kt", [128, 128], BF16, 4)
        km_ring = Ring(K, "km", [128, 128], BF16, 4)
        v32_ring = Ring(K, "v32", [128, 512], F32, 4)
        dtr_ring = Ring(K, "dtr", [128, 8], F32, 4)
        sm_ring = Ring(K, "sm", [128, 8, 8], F32, 4)
        vbf_ring = Ring(K, "vbf", [128, 512], BF16, 4)
        vs_ring = Ring(K, "vs", [128, 512], BF16, 3)
        dec_ring = Ring(K, "dec", [128, 128], F32, 4)
        L_ring = Ring(K, "L", [128, 128], BF16, 4)
        y_ring = Ring(K, "y", [128, 512], F32, 3)
        yt_ring = Ring(K, "ytmp", [128, 512], F32, 3)
        yp_ring = Ring(K, "yprev", [128, 512], F32, 2)
        z_ring = Ring(K, "zt", [128, 512], F32, 2)
        ob_ring = Ring(K, "ob", [128, 512], BF16, 2)
        ost_ring = Ring(K, "ost", [128, 4, 128], BF16, 2)
        st_ring = Ring(K, "stat", [128, 4], F32, 3)
        S32 = K.sb("S32", [128, 512], F32)
        Sbf = K.sb("Sbf", [128, 512], BF16)
        LOOK = 2
        pab = Ring(K, "pab", [128, 512], F32, 1, psum=True)
        pc = Ring(K, "pc", [128, 512], F32, 2, psum=True)
        pY = Ring(K, "pY", [128, 512], F32, LOOK + 1, psum=True)
        pYS = Ring(K, "pYS", [128, 512], F32, 1, psum=True)
        pKV = Ring(K, "pKV", [128, 512], F32, 1, psum=True)

        units = [("ssd", g) for g in range(2)] + [("ret", h) for h in range(4)]
        import os
        if os.environ.get("SCAN_UNITS"):
            units = [units[int(i)] for i in os.environ["SCAN_UNITS"].split(",")]
        ndirs = int(os.environ.get("SCAN_DIRS", "2"))
        for kind, u in units:
            H, P = (8, 64) if kind == "ssd" else (1, 256)
            HP = H * P
            for d in range(ndirs):
                order = list(range(D.nt)) if d == 0 else (list(range(D.ntc - 1, -1, -1)) + list(range(D.nt - 1, D.ntc - 1, -1)))
                U, M = cst[f"U{d}"], cst[f"M{d}"]
                K.memset(K.POOL, S32[:, :HP], 0.0, w=[S32])
                K.memset(K.POOL, Sbf[:, :HP], 0.0, w=[Sbf])
                def front(c):
                    rows = slice(c * 128, (c + 1) * 128)
                    qt, kt, km, v32 = qt_ring.next(), kt_ring.next(), km_ring.next(), v32_ring.next()
                    sm = sm_ring.next()
                    vbf = vbf_ring.next()
                    if kind == "ssd":
                        K.dma(K.SP, qt[:], S["CT"][u * 128:(u + 1) * 128, rows], w=[qt])
                        K.dma(K.ACT, kt[:], S["BT"][u * 128:(u + 1) * 128, rows], w=[kt])
                        K.dma(K.SP, km[:], S["Bm"][rows, u * 128:(u + 1) * 128], w=[km])
                        K.dma(K.ACT, v32[:, :HP], S["XS_tm"][rows, u * 512:(u + 1) * 512], w=[v32])
                        dtr = dtr_ring.next()
                        K.dma(K.SP, dtr[:], S["P_tm"][rows, C_DT + u * 8:C_DT + (u + 1) * 8], w=[dtr])
                        hs = slice(d * 16 + u * 8, d * 16 + u * 8 + 8)
                        dt_ = sm[:, 0, :]
                        K.tt(K.DVE, dt_, dtr[:], dtb[:, hs], ALU.add, r=[dtr, dtb], w=[(id(sm), 0)])
                        K.act(dt_, dt_, AF.Exp, r=[(id(sm), 0)], w=[(id(sm), 0)])
                        K.act(dt_, dt_, AF.Ln, r=[(id(sm), 0)], w=[(id(sm), 0)], bias=1.0)
                        la = sm[:, 1, :]
                        K.tt(K.DVE, la, dt_, negA[:, hs], ALU.mult, r=[(id(sm), 0), negA], w=[(id(sm), 1)])
                        K.tt(K.DVE, vbf[:, :HP].rearrange("p (h q) -> p h q", h=8), v32[:, :HP].rearrange("p (h q) -> p h q", h=8),
                             dt_.unsqueeze(2).to_broadcast([128, 8, 64]), ALU.mult, r=[v32, (id(sm), 0)], w=[vbf])
                        la_key = (id(sm), 1)
                    else:
                        K.dma(K.SP, qt[:], S["QT_r"][u * 128:(u + 1) * 128, rows], w=[qt])
                        K.dma(K.ACT, kt[:], S["KT_r"][u * 128:(u + 1) * 128, rows], w=[kt])
                        K.dma(K.SP, km[:], S["Km_r"][rows, u * 128:(u + 1) * 128], w=[km])
                        K.dma(K.ACT, v32[:, :HP], S["P_tm"][rows, C_V + u * 256:C_V + (u + 1) * 256], w=[v32])
                        K.copy(K.ACT, vbf[:, :HP], v32[:, :HP], r=[v32], w=[vbf])
                        la = laret[:, d * 4 + u:d * 4 + u + 1]
                        la_key = laret
                    pa_ = pab.next()
                    K.mm(pa_[:, 0:H], U[:], la, r=[la_key], w=[pa_])
                    K.mm(pa_[:, 64:64 + H], cst["ones"][:], la, r=[la_key], w=[pa_])
                    negcs, ecs, wdec, elast = sm[:, 2, :H], sm[:, 3, :H], sm[:, 4, :H], sm[:, 5, :H]
                    K.ts(K.DVE, negcs, pa_[:, 0:H], -1.0, ALU.mult, r=[pa_], w=[(id(sm), 2)])
                    K.act(ecs, pa_[:, 0:H], AF.Exp, r=[pa_], w=[(id(sm), 3)])
                    K.tt(K.DVE, wdec, pa_[:, 64:64 + H], negcs, ALU.add, r=[pa_, (id(sm), 2)], w=[(id(sm), 4)])
                    K.act(wdec, wdec, AF.Exp, r=[(id(sm), 4)], w=[(id(sm), 4)])
                    K.act(elast, pa_[:, 64:64 + H], AF.Exp, r=[pa_], w=[(id(sm), 5)])
                    pb_ = pa_
                    K.mm(pb_[:, 128:256], kt[:], qt[:], r=[kt, qt], w=[pb_])
                    pY_ = pY.next()
                    for h in range(H):
                        pc_ = pc.next()
                        sl = slice(0, 128)
                        ck = pc_
                        K.mm(pc_[:, sl], la[:, h:h + 1].to_broadcast([128, 128]), U[:], start=True, stop=False, r=[la_key], w=[ck])
                        K.mm(pc_[:, sl], cst["ident"][:], M[:], start=False, stop=True, w=[ck])
                        dec = dec_ring.next()
                        K.act(dec[:], pc_[:, sl], AF.Exp, r=[ck, (id(sm), 2)], w=[dec], bias=negcs[:, h:h + 1])
                        L = L_ring.next()
                        K.tt(K.DVE, L[:], pb_[:, 128:256], dec[:], ALU.mult, r=[pb_, dec], w=[L])
                        K.mm(pY_[:, h * P:(h + 1) * P], L[:], vbf[:, h * P:(h + 1) * P], r=[L, vbf], w=[pY_])
                    return dict(c=c, rows=rows, qt=qt, km=km, v32=v32, sm=sm, vbf=vbf, pY_=pY_)

                def back(ctx):
                    c, rows, qt, km, v32, sm, vbf, pY_ = (ctx[k] for k in ('c', 'rows', 'qt', 'km', 'v32', 'sm', 'vbf', 'pY_'))
                    ecs, wdec, elast = sm[:, 3, :H], sm[:, 4, :H], sm[:, 5, :H]
                    pYS_ = pYS.next()
                    K.mm(pYS_[:, :HP], qt[:], Sbf[:, :HP], r=[qt, Sbf], w=[pYS_])
                    ytmp = yt_ring.next()
                    y = y_ring.next()
                    K.tt(K.DVE, ytmp[:, :HP].rearrange("p (h q) -> p h q", h=H), pYS_[:, :HP].rearrange("p (h q) -> p h q", h=H),
                         ecs.unsqueeze(2).to_broadcast([128, H, P]), ALU.mult, r=[pYS_, (id(sm), 3)], w=[ytmp])
                    K.tt(K.DVE, y[:, :HP], pY_[:, :HP], ytmp[:, :HP], ALU.add, r=[pY_, ytmp], w=[y])
                    vs = vs_ring.next()
                    K.tt(K.POOL, vs[:, :HP].rearrange("p (h q) -> p h q", h=H), vbf[:, :HP].rearrange("p (h q) -> p h q", h=H),
                         wdec.unsqueeze(2).to_broadcast([128, H, P]), ALU.mult, r=[vbf, (id(sm), 4)], w=[vs])
                    pKV_ = pKV.next()
                    K.mm(pKV_[:, :HP], km[:], vs[:, :HP], r=[km, vs], w=[pKV_])
                    K.tt(K.DVE, S32[:, :HP].rearrange("p (h q) -> p h q", h=H), S32[:, :HP].rearrange("p (h q) -> p h q", h=H),
                         elast.unsqueeze(2).to_broadcast([128, H, P]), ALU.mult, r=[S32, (id(sm), 5), pYS_], w=[S32])
                    K.tt(K.DVE, S32[:, :HP], pKV_[:, :HP], S32[:, :HP], ALU.add, r=[pKV_, S32], w=[S32])
                    K.copy(K.ACT, Sbf[:, :HP], S32[:, :HP], r=[S32], w=[Sbf])
                    yacc = S["YS"] if kind == "ssd" else S["YR"]
                    ycols = slice(u * HP, (u + 1) * HP)
                    ykey = ("yacc", kind, u, c)
                    if d == 0:
                        if kind == "ssd":
                            K.tt(K.POOL, ytmp[:, :HP].rearrange("p (h q) -> p h q", h=H), v32[:, :HP].rearrange("p (h q) -> p h q", h=H),
                                 dsk[:, u * 8:(u + 1) * 8].unsqueeze(2).to_broadcast([128, H, P]), ALU.mult, r=[v32, dsk, y], w=[ytmp])
                            K.tt(K.DVE, y[:, :HP], y[:, :HP], ytmp[:, :HP], ALU.add, r=[y, ytmp], w=[y])
                        K.dma(K.SP, yacc[rows, ycols], y[:, :HP], r=[y], w=[ykey])
                        return
                    yp = yp_ring.next()
                    K.dma(K.SP, yp[:, :HP], yacc[rows, ycols], r=[ykey], w=[yp])
                    K.tt(K.DVE, y[:, :HP], y[:, :HP], yp[:, :HP], ALU.add, r=[y, yp], w=[y])
                    zt = z_ring.next()
                    ob = ob_ring.next()
                    st = st_ring.next()
                    K.memset(K.POOL, st[:], 0.0, w=[st])
                    if kind == "ssd":
                        K.dma(K.ACT, zt[:, :HP], S["P_tm"][rows, C_Z + u * 512:C_Z + (u + 1) * 512], w=[zt])
                        K.act(zt[:, :HP], zt[:, :HP], AF.Silu, r=[zt], w=[zt])
                        K.tt(K.DVE, y[:, :HP], y[:, :HP], zt[:, :HP], ALU.mult, r=[y, zt], w=[y])
                        K.act(ytmp[:, :HP], y[:, :HP], AF.Square, r=[y], w=[ytmp, st], accum_out=st[:, 0:1])
                        rsqrt_mean(K, st[:, 1:2], st[:, 0:1], HP, r=[st], w=[st])
                        K.stt(K.DVE, ob[:, :HP], y[:, :HP], st[:, 1:2], snw[:, ycols], ALU.mult, ALU.mult, r=[y, st, snw], w=[ob])
                        dstT = S["SOT"]
                    else:
                        K.dma(K.ACT, zt[:, :HP], S["P_tm"][rows, C_G + u * 256:C_G + (u + 1) * 256], w=[zt])
                        K.act(zt[:, :HP], zt[:, :HP], AF.Silu, r=[zt], w=[zt])
                        K.op(K.DVE, lambda e: e.reduce_sum(out=st[:, 2:3], in_=y[:, :HP], axis=AX.X), r=[y], w=[st])
                        K.ts(K.DVE, st[:, 2:3], st[:, 2:3], 1.0 / HP, ALU.mult, r=[st], w=[st])
                        K.ts(K.DVE, y[:, :HP], y[:, :HP], st[:, 2:3], ALU.subtract, r=[y, st], w=[y])
                        K.act(ytmp[:, :HP], y[:, :HP], AF.Square, r=[y], w=[ytmp, st], accum_out=st[:, 0:1])
                        rsqrt_mean(K, st[:, 1:2], st[:, 0:1], HP, r=[st], w=[st])
                        K.stt(K.DVE, y[:, :HP], y[:, :HP], st[:, 1:2], gnw[:, ycols], ALU.mult, ALU.mult, r=[y, st, gnw], w=[y])
                        K.tt(K.DVE, ob[:, :HP], y[:, :HP], zt[:, :HP], ALU.mult, r=[y, zt], w=[ob])
                        dstT = S["ROT"]
                    nj = HP // 128
                    pT_ = pab.next()
                    pTb = pT_[:, 256:512].bitcast(BF16)
                    for j in range(nj):
                        K.transpose(pTb[:, j * 128:(j + 1) * 128], ob[:, j * 128:(j + 1) * 128], cst["identb"][:], r=[ob], w=[pT_])
                    ost = ost_ring.next()
                    K.copy(K.ACT, ost[:, :nj, :], pTb[:, :nj * 128].rearrange("p (j q) -> p j q", q=128), r=[pT_], w=[ost])
                    K.dma(K.SP, dstT[u * HP:(u + 1) * HP, rows].rearrange("(j p) t -> p j t", p=128), ost[:, :nj, :], r=[ost])

                pend = []
                for c in order:
                    pend.append(front(c))
                    if len(pend) > LOOK:
                        back(pend.pop(0))
                while pend:
                    back(pend.pop(0))


def build_outproj(K, D, I, l, S, cst, modT, deltaT):
    with K.scope():
        ws = {}
        for nm in ("wsso", "wro", "wo"):
            ws[nm] = K.sb(nm, [128, 8, 1024], BF16)
            K.dma(K.POOL, ws[nm][:], I[f"{nm}{l}"].rearrange("(k p) c -> p k c", p=128), w=[ws[nm]])
        sot_ring = Ring(K, "sot", [128, 8, 512], BF16, 2)
        rot_ring = Ring(K, "rot", [128, 8, 512], BF16, 2)
        mT_ring = Ring(K, "mT", [128, 8, 512], BF16, 2)
        sg_ring = Ring(K, "sg", [128, 2, 512], F32, 3)
        m_ring = Ring(K, "mtmp", [128, 2, 512], F32, 3)
        st_ring = Ring(K, "ostg", [128, 512], F32, 3)
        pring = Ring(K, "pop", [128, 512], F32, 6, psum=True)
        for (s0, n, r) in D.blocks:
            sot, rot, mT = sot_ring.next(), rot_ring.next(), mT_ring.next()
            K.dma(K.SP, sot[:, :, :n], S["SOT"][:, s0:s0 + n].rearrange("(k p) t -> p k t", p=128), w=[sot])
            K.dma(K.ACT, rot[:, :, :n], S["ROT"][:, s0:s0 + n].rearrange("(k p) t -> p k t", p=128), w=[rot])
            for dc in range(8):
                ps1, ps2 = pring.next(), pring.next()
                for k in range(8):
                    K.mm(ps1[:, :n], ws["wsso"][:, k, dc * 128:(dc + 1) * 128], sot[:, k, :n], start=(k == 0), stop=(k == 7),
                         r=[ws["wsso"], sot], w=[ps1])
                for k in range(8):
                    K.mm(ps2[:, :n], ws["wro"][:, k, dc * 128:(dc + 1) * 128], rot[:, k, :n], start=(k == 0), stop=(k == 7),
                         r=[ws["wro"], rot], w=[ps2])
                sg = sg_ring.next()
                K.dma(K.SP, sg[:, 0, :n], S["PT_fm"][(12 + dc) * 128:(13 + dc) * 128, s0:s0 + n], w=[sg])
                K.dma(K.ACT, sg[:, 1, :n], S["PT_fm"][(20 + dc) * 128:(21 + dc) * 128, s0:s0 + n], w=[sg])
                mt = m_ring.next()
                K.tt(K.DVE, mt[:, 0, :n], ps1[:, :n], sg[:, 0, :n], ALU.mult, r=[ps1, sg], w=[(id(mt), 0)])
                K.tt(K.DVE, mt[:, 1, :n], ps2[:, :n], sg[:, 1, :n], ALU.mult, r=[ps2, sg], w=[(id(mt), 1)])
                K.tt(K.POOL, mT[:, dc, :n], mt[:, 0, :n], mt[:, 1, :n], ALU.add, r=[(id(mt), 0), (id(mt), 1)], w=[(id(mT), dc)])
            for dc in range(8):
                ps = pring.next()
                for k in range(8):
                    K.mm(ps[:, :n], ws["wo"][:, k, dc * 128:(dc + 1) * 128], mT[:, k, :n], start=(k == 0), stop=(k == 7),
                         r=[ws["wo"]] + [(id(mT), kk) for kk in range(8)], w=[ps])
                st = st_ring.next()
                K.ts(K.DVE, st[:, :n], ps[:, :n], modT[:, 16 + dc, r:r + 1], ALU.mult, r=[ps, modT], w=[st])
                K.dma(K.SP, deltaT[dc * 128:(dc + 1) * 128, s0:s0 + n], st[:, :n], r=[st], w=[("delta", dc, s0)])


def alloc_mixer_scratch(nc, D, tag=""):
    T = D.T
    def dt_(name, shape, dtype):
        return nc.dram_tensor(name + tag, list(shape), dtype, kind="Internal").ap()
    return {
        "P_tm": dt_("P_tm", [T, N_TM], F32), "PT_fm": dt_("PT_fm", [N_FM, T], F32),
        "XS_tm": dt_("XS_tm", [T, 1024], F32), "BT": dt_("BT", [256, T], BF16), "CT": dt_("CT", [256, T], BF16),
        "Bm": dt_("Bm", [T, 256], BF16), "Km_r": dt_("Km_r", [T, 512], BF16),
        "QT_r": dt_("QT_r", [512, T], BF16), "KT_r": dt_("KT_r", [512, T], BF16),
        "YS": dt_("YS", [T, 1024], F32), "YR": dt_("YR", [T, 1024], F32),
        "SOT": dt_("SOT", [1024, T], BF16), "ROT": dt_("ROT", [1024, T], BF16),
    }


def mixer_input_specs(D, l):
    return {
        f"w_mod{l}": ([1024, 6144], F32), f"b_modT{l}": ([128, 48], F32), f"nmw{l}": ([128, 8], F32), f"nfw{l}": ([128, 8], F32),
        f"w_core{l}": ([1024, N_TM + N_FM], F32), f"cw{l}": ([128, 12, 3], F32), f"cbias{l}": ([128, 12], F32),
        f"dtb{l}": ([1, 32], F32), f"alog{l}": ([1, 32], F32), f"dsk{l}": ([1, 16], F32), f"snw{l}": ([1, 1024], F32),
        f"rdec{l}": ([1, 8], F32), f"gnw{l}": ([1, 1024], F32),
        f"wsso{l}": ([1024, 1024], F32), f"wro{l}": ([1024, 1024], F32), f"wo{l}": ([1024, 1024], F32),
    }


def build_mixer_layer(K, nc, D, I, l, S, cst, modT, x_srcs, deltaT, xsum_out=None, nphase=99):
    build_mod(K, nc, D, I, l, modT, cst)
    if nphase < 2:
        return
    with K.scope():
        hlT = K.sb("hlT", [128, 8, D.T], BF16)
        nmw = K.sb("nmw", [128, 8], F32)
        K.dma(K.SP, nmw[:], I[f"nmw{l}"], w=[nmw])
        build_norm_mod(K, D, x_srcs, nmw, modT, 0, 1, hlT, cst, xsum_out=xsum_out)
        if nphase >= 3:
            build_inproj(K, D, I, l, hlT, S, cst)
    if nphase >= 4:
        build_conv(K, D, I, l, S, cst)
    if nphase >= 5:
        build_rope(K, D, I, S, cst)
    if nphase >= 6:
        build_scan(K, D, I, l, S, cst)
    if nphase >= 7:
        build_outproj(K, D, I, l, S, cst, modT, deltaT)


def rope_tables(TL, grid_w=64):
    rows = TL // grid_w
    r, col = np.meshgrid(np.arange(rows), np.arange(grid_w), indexing="ij")
    n_freq = 32
    inv = (np.float32(10000.0) ** (-np.arange(n_freq, dtype=np.float32) / np.float32(n_freq))).astype(np.float32)
    ang = np.concatenate([r.reshape(-1, 1).astype(np.float32) * inv, col.reshape(-1, 1).astype(np.float32) * inv], axis=-1)
    return np.cos(ang).astype(np.float32), np.sin(ang).astype(np.float32)


def fm(v, nchunk):
    return np.ascontiguousarray(np.asarray(v, np.float32).reshape(nchunk, 128).T)


def prep_mixer_inputs(inp, l, s):
    w_in = np.asarray(inp["w_in"][l], np.float32)
    o_z, o_x, o_dt, o_q, o_k, o_v, o_g, o_gs, o_gr = 0, 2048, 5120, 5152, 6176, 7200, 9248, 11296, 12320
    cols_tm = [w_in[:, o_z + s * 1024:o_z + (s + 1) * 1024], w_in[:, o_q + s * 512:o_q + (s + 1) * 512],
               w_in[:, o_k + s * 512:o_k + (s + 1) * 512], w_in[:, o_v + s * 1024:o_v + (s + 1) * 1024],
               w_in[:, o_g + s * 1024:o_g + (s + 1) * 1024], w_in[:, o_dt + s * 16:o_dt + (s + 1) * 16],
               np.zeros((1024, 496), np.float32)]
    xs_c = np.arange(s * 1024, (s + 1) * 1024)
    b_c = 2048 + np.arange(s * 256, (s + 1) * 256)
    c_c = 2560 + np.arange(s * 256, (s + 1) * 256)
    xbc_ch = np.concatenate([xs_c, b_c, c_c])
    cols_fm = [w_in[:, o_x + xbc_ch], w_in[:, o_gs:o_gs + 1024], w_in[:, o_gr:o_gr + 1024]]
    w_core = np.ascontiguousarray(np.concatenate(cols_tm + cols_fm, axis=1))
    cw = np.asarray(inp["conv_w"][l], np.float32)[:, xbc_ch]
    cwT = np.ascontiguousarray(cw.reshape(3, 12, 128).transpose(2, 1, 0))
    cbT = fm(np.asarray(inp["conv_b"][l], np.float32)[xbc_ch], 12)
    hsl = slice(s * 16, (s + 1) * 16)
    out = {
        f"w_mod{l}": np.ascontiguousarray(inp["w_mod"][l], np.float32), f"b_modT{l}": fm(inp["b_mod"][l], 48),
        f"nmw{l}": fm(inp["norm_mix_w"][l], 8), f"nfw{l}": fm(inp["norm_ffn_w"][l], 8),
        f"w_core{l}": w_core, f"cw{l}": cwT, f"cbias{l}": cbT,
        f"dtb{l}": np.ascontiguousarray(np.asarray(inp["ssd_dt_bias"][l], np.float32)[:, hsl].reshape(1, 32)),
        f"alog{l}": np.ascontiguousarray(np.asarray(inp["ssd_a_log"][l], np.float32)[:, hsl].reshape(1, 32)),
        f"dsk{l}": np.ascontiguousarray(np.asarray(inp["ssd_d"][l], np.float32)[hsl].reshape(1, 16)),
        f"snw{l}": np.ascontiguousarray(np.asarray(inp["ssd_norm_w"][l], np.float32)[s * 1024:(s + 1) * 1024].reshape(1, 1024)),
        f"rdec{l}": np.ascontiguousarray(np.asarray(inp["ret_decay"][l], np.float32)[:, s * 4:(s + 1) * 4].reshape(1, 8)),
        f"gnw{l}": np.ascontiguousarray(np.asarray(inp["ret_gn_w"][l], np.float32)[s * 1024:(s + 1) * 1024].reshape(1, 1024)),
        f"wsso{l}": np.ascontiguousarray(np.asarray(inp["w_ssd_o"][l], np.float32)[s * 1024:(s + 1) * 1024]),
        f"wro{l}": np.ascontiguousarray(np.asarray(inp["w_ret_o"][l], np.float32)[s * 1024:(s + 1) * 1024]),
        f"wo{l}": np.ascontiguousarray(inp["w_o"][l], np.float32),
    }
    return out


def prep_common_inputs(inp, b, D):
    xfull = np.concatenate([np.asarray(inp["ctx"][b], np.float32), np.asarray(inp["x"][b], np.float32)], axis=0)
    cond = np.stack([np.asarray(inp["c"][b], np.float32), np.asarray(inp["c_ctx"], np.float32)], axis=-1)
    cos, sin = rope_tables(D.TL)
    return {"xT": np.ascontiguousarray(xfull.T), "condT": np.ascontiguousarray(cond.reshape(8, 128, 2).transpose(1, 0, 2)),
            "cos": cos, "sin": sin}


ROWW = 1024 + 32
BIGPOS = 1.0e6


def alloc_ffn_scratch(nc, D, n_exp, tag=""):
    def dt_(name, shape, dtype):
        return nc.dram_tensor(name + tag, list(shape), dtype, kind="Internal").ap()
    capl, capc = D.TL // 8, D.TC // 8
    return {"HL2": dt_("HL2", [D.T, ROWW], I16), "AFFT": dt_("AFFT", [16, D.T], F32),
            "XINL": [dt_(f"XINL{i}", [capl, ROWW], I16) for i in range(n_exp)],
            "XINC": [dt_(f"XINC{i}", [capc, ROWW], I16) for i in range(n_exp)],
            "OUTL": [dt_(f"OUTL{i}", [capl, 1024], F32) for i in range(n_exp)],
            "OUTC": [dt_(f"OUTC{i}", [capc, 1024], F32) for i in range(n_exp)]}


def ffn_input_specs(l, n_exp):
    return {f"w_router{l}": ([1024, 16], F32), f"wg{l}": ([n_exp, 1024, 2048], F32), f"wu{l}": ([n_exp, 1024, 2048], F32),
            f"wd{l}": ([n_exp, 2048, 1024], F32)}


def build_ffn_layer(K, nc, D, I, l, F, cst, modT, x_srcs, X1T, outT, exp_ids, do_ctx, out_scale):
    n_exp = len(exp_ids)
    sets = [("L", D.ntc, D.nt, D.TL // 8)] + ([("C", 0, D.ntc, D.TC // 8)] if do_ctx else [])
    with K.scope():
        POS = K.sb("POS", [128, D.nt, 16], I32)
        with K.scope():
            hlT = K.sb("hl2T", [128, 8, D.T], BF16)
            nfw = K.sb("nfw", [128, 8], F32)
            K.dma(K.SP, nfw[:], I[f"nfw{l}"], w=[nfw])
            build_norm_mod(K, D, x_srcs, nfw, modT, 3, 4, hlT, cst, xsum_out=X1T)
            wr32 = K.sb("wr32", [128, 8, 16], F32)
            K.dma(K.SP, wr32[:], I[f"w_router{l}"].rearrange("(k p) e -> p k e", p=128), w=[wr32])
            wrb = K.sb("wrb", [128, 8, 16], BF16)
            K.copy(K.DVE, wrb[:], wr32[:], r=[wr32], w=[wrb])
            aff_ring = Ring(K, "affb", [16, 512], F32, 2)
            e_ring = Ring(K, "eexp", [16, 512], F32, 2)
            rs_ring = Ring(K, "rsum", [16, 512], F32, 2)
            row_ring = Ring(K, "row", [128, ROWW], I16, 3)
            pl = Ring(K, "plog", [128, 512], F32, 2, psum=True)
            pt = Ring(K, "ptr", [128, 512], F32, 3, psum=True)
            for (s0, n, r) in D.blocks:
                ps = pl.next()
                for k in range(8):
                    K.mm(ps[0:16, :n], wrb[:, k, :], hlT[:, k, s0:s0 + n], start=(k == 0), stop=(k == 7), r=[wrb], w=[ps])
                ee = e_ring.next()
                K.act(ee[:, :n], ps[0:16, :n], AF.Exp, r=[ps], w=[ee])
                ps2 = pl.next()
                K.mm(ps2[0:16, :n], cst["ones"][0:16, 0:16], ee[:, :n], r=[ee], w=[ps2])
                rs = rs_ring.next()
                K.op(K.DVE, lambda e_: e_.reciprocal(out=rs[:, :n], in_=ps2[0:16, :n]), r=[ps2], w=[rs])
                affb = aff_ring.next()
                K.tt(K.DVE, affb[:, :n], ee[:, :n], rs[:, :n], ALU.mult, r=[ee, rs], w=[affb])
                K.dma(K.ACT, F["AFFT"][:, s0:s0 + n], affb[:, :n], r=[affb])
                for j in range(n // 128):
                    t0 = s0 + j * 128
                    row = row_ring.next()
                    for half in range(2):
                        pp = pt.next()
                        ppb = pp[:].bitcast(BF16)
                        for kk in range(4):
                            k = half * 4 + kk
                            K.transpose(ppb[:, kk * 128:(kk + 1) * 128], hlT[:, k, t0:t0 + 128], cst["identb"][:], w=[pp])
                        K.copy(K.ACT if half == 0 else K.DVE, row[:, half * 512:(half + 1) * 512].bitcast(BF16), ppb[:, 0:512], r=[pp], w=[(id(row), half)])
                    pa = pt.next()
                    K.transpose(pa[:, 0:16], affb[:, j * 128:(j + 1) * 128], cst["ident"][0:16, 0:16], r=[affb], w=[pa])
                    K.copy(K.DVE, row[:, 1024:ROWW].bitcast(F32), pa[:, 0:16], r=[pa], w=[(id(row), 2)])
                    K.dma(K.SP, F["HL2"][t0:t0 + 128, :], row[:], r=[(id(row), 0), (id(row), 1), (id(row), 2)])
        with K.scope():
            affT = K.sb("affT", [16, D.T], F32)
            K.dma(K.SP, affT[:], F["AFFT"], w=[affT])
            bs = K.sb("bis", [16, 8], F32)
            junk = K.sb("junk", [16, D.TL], F32)
            msk_ring = Ring(K, "msk", [16, 128], F32, 2)
            mtm_ring = Ring(K, "mtm", [128, 16], F32, 2)
            accm = K.sb("accm", [128, 16], F32)
            pf_ring = Ring(K, "posf", [128, 16], F32, 2)
            pp_ring = Ring(K, "ppos", [128, 512], F32, 2, psum=True)
            pm_ring = Ring(K, "pmsk", [128, 512], F32, 2, psum=True)
            K.memset(K.DVE, POS[:], 0, w=[POS])
            for (sname, ta, tb, cap) in sets:
                c0, c1 = ta * 128, tb * 128
                K.memset(K.DVE, bs[:, 0:1], 0.0, w=[bs])
                K.memset(K.DVE, bs[:, 1:2], 1.0, w=[bs])
                for it in range(30):
                    K.tt(K.DVE, bs[:, 2:3], bs[:, 0:1], bs[:, 1:2], ALU.add, r=[bs], w=[bs])
                    K.ts(K.DVE, bs[:, 2:3], bs[:, 2:3], 0.5, ALU.mult, r=[bs], w=[bs])
                    K.memset(K.DVE, bs[:, 3:4], 0.0, w=[bs])
                    K.ts(K.DVE, junk[:, :c1 - c0], affT[:, c0:c1], bs[:, 2:3], ALU.is_ge, 0.0, ALU.add, r=[bs, affT], w=[bs, junk],
                         accum_out=bs[:, 3:4])
                    K.ts(K.DVE, bs[:, 4:5], bs[:, 3:4], float(cap), ALU.is_ge, r=[bs], w=[bs])
                    K.tt(K.DVE, bs[:, 5:6], bs[:, 2:3], bs[:, 0:1], ALU.subtract, r=[bs], w=[bs])
                    K.stt(K.DVE, bs[:, 0:1], bs[:, 5:6], bs[:, 4:5], bs[:, 0:1], ALU.mult, ALU.add, r=[bs], w=[bs])
                    K.tt(K.DVE, bs[:, 5:6], bs[:, 1:2], bs[:, 2:3], ALU.subtract, r=[bs], w=[bs])
                    K.stt(K.DVE, bs[:, 1:2], bs[:, 5:6], bs[:, 4:5], bs[:, 2:3], ALU.mult, ALU.add, r=[bs], w=[bs])
                K.memset(K.POOL, accm[:], 0.0, w=[accm])
                for t in range(ta, tb):
                    mk = msk_ring.next()
                    K.ts(K.DVE, mk[:], affT[:, t * 128:(t + 1) * 128], bs[:, 0:1], ALU.is_ge, r=[bs, affT], w=[mk])
                    pm = pm_ring.next()
                    K.transpose(pm[:, 0:16], mk[:], cst["ident"][0:16, 0:16], r=[mk], w=[pm])
                    mtm = mtm_ring.next()
                    K.copy(K.ACT, mtm[:], pm[:, 0:16], r=[pm], w=[mtm])
                    pp = pp_ring.next()
                    K.mm(pp[:, 0:16], cst["U0"][:], mtm[:], start=True, stop=False, r=[mtm], w=[pp])
                    K.mm(pp[:, 0:16], cst["ones"][:], accm[:], start=False, stop=True, r=[accm], w=[pp])
                    pf = pf_ring.next()
                    K.ts(K.DVE, pf[:], pp[:, 0:16], -1.0 - BIGPOS, ALU.add, r=[pp], w=[pf])
                    K.tt(K.DVE, pf[:], pf[:], mtm[:], ALU.mult, r=[pf, mtm], w=[pf])
                    K.ts(K.DVE, pf[:], pf[:], BIGPOS, ALU.add, r=[pf], w=[pf])
                    K.copy(K.DVE, POS[:, t, :], pf[:], r=[pf], w=[("POS", t)])
                    K.tt(K.POOL, accm[:], accm[:], mtm[:], ALU.add, r=[accm, mtm], w=[accm])
        with K.scope():
            row_ring = Ring(K, "drow", [128, ROWW], I16, 3)
            for (sname, ta, tb, cap) in sets:
                XIN = F["XINL"] if sname == "L" else F["XINC"]
                for t in range(ta, tb):
                    row = row_ring.next()
                    K.dma(K.SP, row[:], F["HL2"][t * 128:(t + 1) * 128, :], w=[row])
                    for ei, e in enumerate(exp_ids):
                        K.dma(K.POOL, lambda q, ei=ei, e=e, t=t, row=row, XIN=XIN, cap=cap: q.indirect_dma_start(
                            out=XIN[ei], out_offset=bass.IndirectOffsetOnAxis(ap=POS[:, t, e:e + 1], axis=0),
                            in_=row[:], in_offset=None, bounds_check=K.bound_reg(cap - 1), oob_is_err=False),
                            None, r=[row, ("POS", t)], w=[("XIN", sname, ei)])
        with K.scope():
            wring = Ring(K, "wexp", [128, 16384], BF16, 3)
            xin_ring = Ring(K, "xin", [128, ROWW], I16, 3)
            xinT = K.sb("xinT", [128, 8, 1024], BF16)
            hidT = K.sb("hidT", [128, 16, 1024], BF16)
            gs_ring = Ring(K, "gsel", [128, 8, 16], F32, 2)
            sg_ring = Ring(K, "sgate", [128, 512], F32, 2)
            o_ring = Ring(K, "oexp", [128, 1024], F32, 2)
            pg = Ring(K, "pg", [128, 512], F32, 2, psum=True)
            pu = Ring(K, "pu", [128, 512], F32, 2, psum=True)
            po = Ring(K, "po", [128, 512], F32, 2, psum=True)
            px = Ring(K, "px", [128, 512], F32, 2, psum=True)
            for ei, e in enumerate(exp_ids):
                wg_, wu_, wd_ = wring.next(), wring.next(), wring.next()
                wg = wg_[:].rearrange("p (k f) -> p k f", k=8)
                wu = wu_[:].rearrange("p (k f) -> p k f", k=8)
                wd = wd_[:].rearrange("p (j d) -> p j d", j=16)
                K.dma(K.POOL, wg, I[f"wg{l}"][ei].rearrange("(k p) f -> p k f", p=128), w=[wg_])
                K.dma(K.POOL, wu, I[f"wu{l}"][ei].rearrange("(k p) f -> p k f", p=128), w=[wu_])
                K.dma(K.POOL, wd, I[f"wd{l}"][ei].rearrange("(j p) d -> p j d", p=128), w=[wd_])
                for (sname, ta, tb, cap) in sets:
                    XIN = F["XINL"] if sname == "L" else F["XINC"]
                    OUT = F["OUTL"] if sname == "L" else F["OUTC"]
                    nst = (cap + 127) // 128
                    gs = gs_ring.next()
                    for st in range(nst):
                        ns = min(128, cap - st * 128)
                        xin = xin_ring.next()
                        K.dma(K.SP, xin[:ns, :], XIN[ei][st * 128:st * 128 + ns, :], r=[("XIN", sname, ei)], w=[xin])
                        K.copy(K.DVE, gs[:ns, st, :], xin[:ns, 1024:ROWW].bitcast(F32), r=[xin], w=[gs])
                        for half in range(2):
                            pp = px.next()
                            ppb = pp[:].bitcast(BF16)
                            for kk in range(4):
                                k = half * 4 + kk
                                K.transpose(ppb[:, kk * 128:kk * 128 + ns], xin[:ns, k * 128:(k + 1) * 128].bitcast(BF16), cst["identb"][:ns, :ns], r=[xin], w=[pp])
                            K.copy(K.ACT if half == 0 else K.DVE, xinT[:, half * 4:(half + 1) * 4, st * 128:st * 128 + ns],
                                   ppb[:, 0:512].rearrange("p (k q) -> p k q", q=128)[:, :, :ns], r=[pp], w=[("xinT", st)])
                    sblocks = [(b0, min(512, cap - b0)) for b0 in range(0, cap, 512)]
                    for j in range(16):
                        for (b0, bn) in sblocks:
                            rk = [("xinT", st) for st in range(b0 // 128, (b0 + bn + 127) // 128)]
                            g_, u_ = pg.next(), pu.next()
                            for k in range(8):
                                K.mm(g_[:, :bn], wg[:, k, j * 128:(j + 1) * 128], xinT[:, k, b0:b0 + bn], start=(k == 0), stop=(k == 7), r=[wg_] + rk, w=[g_])
                            for k in range(8):
                                K.mm(u_[:, :bn], wu[:, k, j * 128:(j + 1) * 128], xinT[:, k, b0:b0 + bn], start=(k == 0), stop=(k == 7), r=[wu_] + rk, w=[u_])
                            sg = sg_ring.next()
                            K.act(sg[:, :bn], g_[:, :bn], AF.Silu, r=[g_], w=[sg])
                            K.tt(K.DVE, hidT[:, j, b0:b0 + bn], u_[:, :bn], sg[:, :bn], ALU.mult, r=[u_, sg], w=[("hidT", j, b0)])
                    for st in range(nst):
                        ns = min(128, cap - st * 128)
                        ob = o_ring.next()
                        for dh in range(2):
                            o_ = po.next()
                            for j in range(16):
                                K.mm(o_[:ns, :], hidT[:, j, st * 128:st * 128 + ns], wd[:, j, dh * 512:(dh + 1) * 512], start=(j == 0), stop=(j == 15),
                                     r=[wd_] + [("hidT", j, (st * 128) // 512 * 512)], w=[o_])
                            K.ts(K.DVE, ob[:ns, dh * 512:(dh + 1) * 512], o_[:ns, :], gs[:ns, st, e:e + 1], ALU.mult, r=[o_, gs], w=[(id(ob), dh)])
                        K.dma(K.SP, OUT[ei][st * 128:st * 128 + ns, :], ob[:ns, :], r=[(id(ob), 0), (id(ob), 1)], w=[("OUT", sname, ei)])
        with K.scope():
            g_ring = Ring(K, "gath", [128, 1024], F32, 4)
            acc_ring = Ring(K, "cacc", [128, 1024], F32, 2)
            x1_ring = Ring(K, "x1t", [128, 8, 128], F32, 2)
            o_ring = Ring(K, "x2t", [128, 8, 128], F32, 2)
            ptr = Ring(K, "pct", [128, 512], F32, 4, psum=True)
            moe_tiles = set()
            for (sname, ta, tb, cap) in sets:
                moe_tiles.update(range(ta, tb))
            for t in range(D.nt):
                r = 1 if t < D.ntc else 0
                x1 = x1_ring.next()
                K.dma(K.SP, x1[:], X1T[:, t * 128:(t + 1) * 128].rearrange("(k p) t -> p k t", p=128), w=[x1])
                xo = o_ring.next()
                if t not in moe_tiles:
                    K.ts(K.DVE, xo[:], x1[:], float(out_scale), ALU.mult, r=[x1], w=[xo])
                else:
                    sname = "C" if t < D.ntc else "L"
                    cap = D.TC // 8 if sname == "C" else D.TL // 8
                    OUT = F["OUTL"] if sname == "L" else F["OUTC"]
                    acc = acc_ring.next()
                    K.memset(K.DVE, acc[:], 0.0, w=[acc])
                    for ei, e in enumerate(exp_ids):
                        g = g_ring.next()
                        K.memset(K.POOL, g[:], 0.0, w=[g])
                        K.dma(K.POOL, lambda q, ei=ei, e=e, t=t, g=g, OUT=OUT, cap=cap: q.indirect_dma_start(
                            out=g[:], out_offset=None, in_=OUT[ei],
                            in_offset=bass.IndirectOffsetOnAxis(ap=POS[:, t, e:e + 1], axis=0),
                            bounds_check=K.bound_reg(cap - 1), oob_is_err=False),
                            None, r=[("OUT", sname, ei)], w=[g])
                        K.tt(K.DVE, acc[:], acc[:], g[:], ALU.add, r=[acc, g], w=[acc])
                    for half in range(2):
                        pp = ptr.next()
                        for kk in range(4):
                            k = half * 4 + kk
                            K.transpose(pp[:, kk * 128:(kk + 1) * 128], acc[:, k * 128:(k + 1) * 128], cst["ident"][:], r=[acc], w=[pp])
                        for kk in range(4):
                            k = half * 4 + kk
                            K.ts(K.DVE, xo[:, k, :], pp[:, kk * 128:(kk + 1) * 128], modT[:, 40 + k, r:r + 1], ALU.mult, r=[pp, modT], w=[xo])
                    K.stt(K.DVE, xo[:], x1[:], float(out_scale), xo[:], ALU.mult, ALU.add, r=[x1, xo], w=[xo])
                K.dma(K.SP, outT[:, t * 128:(t + 1) * 128].rearrange("(k p) t -> p k t", p=128), xo[:], r=[xo], w=[("outT", t)])


PER_S = ("w_core", "cw", "cbias", "dtb", "alog", "dsk", "snw", "rdec", "gnw", "wsso", "wro")
SHARED = ("w_mod", "b_modT", "nmw", "nfw", "wo")
DEPTH_ = 2


def build_final(K, D, I, cst, XT, outT):
    with K.scope():
        fw = K.sb("fw", [128, 8], F32)
        K.dma(K.SP, fw[:], I["fnw"], w=[fw])
        xring = Ring(K, "fxb", [128, 8, 512], F32, 2)
        sq = K.sb("fsq", [128, 8, 512], F32)
        rstd = K.sb("frstd", [128, 512], F32)
        oring = Ring(K, "fo", [128, 8, 512], F32, 2)
        pring = Ring(K, "fpn", [128, 512], F32, 2, psum=True)
        for (s0, n, r) in D.blocks[1:]:
            xb = xring.next()
            K.dma(K.SP, xb[:], XT[:, s0:s0 + n].rearrange("(k p) t -> p k t", p=128), w=[xb])
            K.act(sq[:], xb[:], AF.Square, r=[xb], w=[sq])
            ps = pring.next()
            for k in range(8):
                K.mm(ps[:], cst["ones"][:], sq[:, k, :], start=(k == 0), stop=(k == 7), r=[sq], w=[ps])
            rsqrt_mean(K, rstd[:], ps[:], 1024, r=[ps], w=[rstd])
            ob = oring.next()
            for k in range(8):
                K.stt(K.DVE, ob[:, k, :], xb[:, k, :], fw[:, k:k + 1], rstd[:], ALU.mult, ALU.mult,
                      r=[xb, rstd, fw], w=[(id(ob), k)])
            K.dma(K.ACT, outT[:, s0 - D.TC:s0 - D.TC + n].rearrange("(k p) t -> p k t", p=128), ob[:],
                  r=[(id(ob), k) for k in range(8)], is_output=True)


def full_input_specs(D):
    specs = {"xT": ([1024, D.T], F32), "condT": ([128, 8, 2], F32), "cos": ([D.TL, 64], F32), "sin": ([D.TL, 64], F32),
             "fnw": ([128, 8], F32)}
    for l in range(DEPTH_):
        ms = mixer_input_specs(D, l)
        for base in SHARED:
            specs[f"{base}{l}"] = ms[f"{base}{l}"]
        for s in range(2):
            for base in PER_S:
                specs[f"{base}{l}s{s}"] = ms[f"{base}{l}"]
        specs.update(ffn_input_specs(l, 16))
    return specs


def build_full(D):
    nc = bass.Bass("TRN2", target_bir_lowering=False)
    I = {k: nc.dram_tensor(k, shp, dt, kind="ExternalInput").ap() for k, (shp, dt) in full_input_specs(D).items()}
    outT = nc.dram_tensor("outT", [1024, D.TL], F32, kind="ExternalOutput").ap()
    def dram(name):
        return nc.dram_tensor(name, [1024, D.T], F32, kind="Internal").ap()
    K = KB(nc)
    S = alloc_mixer_scratch(nc, D)
    F = alloc_ffn_scratch(nc, D, 16)
    cst = make_consts(K)
    modT = K.sb("modT", [128, 48, 2], F32)
    XT = I["xT"]
    deltas = [dram("DELTA0"), dram("DELTA1")]
    X1T = dram("X1T")
    xnext = [dram("XN0"), dram("XN1")]
    for l in range(DEPTH_):
        for s in range(2):
            tag = f"{l}s{s}"
            Iv = dict(I)
            for base in SHARED:
                Iv[f"{base}{tag}"] = I[f"{base}{l}"]
            build_mixer_layer(K, nc, D, Iv, tag, S, cst, modT, [XT], deltas[s])
        last = (l == DEPTH_ - 1)
        build_ffn_layer(K, nc, D, I, l, F, cst, modT, [XT] + deltas, X1T, xnext[l], list(range(16)), not last, 1.0)
        XT = xnext[l]
    build_final(K, D, I, cst, XT, outT)
    K.finish()
    return nc, K


def prep_core_inputs(inp, b, D):
    m = prep_common_inputs(inp, b, D)
    m["fnw"] = fm(inp["final_norm_w"], 8)
    for l in range(DEPTH_):
        for s in range(2):
            pm = prep_mixer_inputs(inp, l, s)
            for base in SHARED:
                m[f"{base}{l}"] = pm[f"{base}{l}"]
            for base in PER_S:
                m[f"{base}{l}s{s}"] = pm[f"{base}{l}"]
        m[f"w_router{l}"] = np.ascontiguousarray(inp["w_router"][l], np.float32)
        m[f"wg{l}"] = np.ascontiguousarray(inp["w_gate"][l], np.float32)
        m[f"wu{l}"] = np.ascontiguousarray(inp["w_up"][l], np.float32)
        m[f"wd{l}"] = np.ascontiguousarray(inp["w_down"][l], np.float32)
    return m


def kernel(**inputs):
    inp = {k: np.asarray(v) for k, v in inputs.items()}
    B = inp["x"].shape[0]
    D = Dims(inp["ctx"].shape[1] // 128, inp["x"].shape[1] // 128)
    nc, _ = build_full(D)
    in_maps = [prep_core_inputs(inp, b, D) for b in range(B)]
    res = run_bass_kernel_spmd(nc, in_maps, core_ids=list(range(B)))
    out = np.stack([np.ascontiguousarray(res.results[b]["outT"].T) for b in range(B)], axis=0)
    return out.astype(np.float32)
```

```python
import contextlib
import numpy as np
import concourse.bass as bass
import concourse.mybir as mybir
from concourse.bass_utils import run_bass_kernel_spmd

F32 = mybir.dt.float32
BF16 = mybir.dt.bfloat16
I32 = mybir.dt.int32
I16 = mybir.dt.int16
AF = mybir.ActivationFunctionType
ALU = mybir.AluOpType
AX = mybir.AxisListType


class KB:
    N_DMA_SEMS = 40

    def __init__(self, nc):
        self.nc = nc
        self.stack = contextlib.ExitStack()
        self.stack0 = self.stack
        self.PE, self.ACT, self.DVE, self.POOL, self.SP = nc.tensor, nc.scalar, nc.vector, nc.gpsimd, nc.sync
        self.engs = [self.PE, self.ACT, self.DVE, self.POOL, self.SP]
        self.esem = {}
        self.ecnt = {}
        for i, e in enumerate(self.engs):
            self.esem[id(e)] = self.stack.enter_context(nc.semaphore(f"es{i}"))
            self.ecnt[id(e)] = 0
        self.dsems = [self.stack.enter_context(nc.semaphore(f"ds{i}")) for i in range(self.N_DMA_SEMS)]
        self.dcnt = [0] * self.N_DMA_SEMS
        self.dnext = 0
        self.seen = {id(e): {} for e in self.engs}
        self.semobj = {}
        self.lastw = {}
        self.reads = {}
        self.out_events = []
        self.n_inst = 0
        self.n_wait = 0

    def _uname(self, name):
        self.uid = getattr(self, "uid", 0) + 1
        return f"{name}_u{self.uid}"

    def sb(self, name, shape, dtype):
        return self.stack.enter_context(self.nc.sbuf_tensor(self._uname("s_" + name), list(shape), dtype))

    def ps(self, name, shape, dtype=F32):
        return self.stack.enter_context(self.nc.psum_tensor(self._uname("p_" + name), list(shape), dtype))

    @staticmethod
    def _key(b):
        return b if isinstance(b, (tuple, str, int)) else id(b)

    def _wait(self, eng, ev):
        sem, val = ev
        seen = self.seen[id(eng)]
        if seen.get(id(sem), 0) >= val:
            return
        eng.wait_ge(sem, val)
        self.n_wait += 1
        seen[id(sem)] = val

    def _deps(self, eng, r, w, same_engine_ok=False):
        evs = []
        for b in list(r) + list(w):
            ev = self.lastw.get(self._key(b))
            if ev is not None:
                evs.append(ev)
        for b in w:
            evs.extend(self.reads.get(self._key(b), []))
        mysem = self.esem[id(eng)]
        for ev in evs:
            if same_engine_ok and ev[0] is mysem:
                continue
            self._wait(eng, ev)

    def _record(self, ev, r, w):
        for b in r:
            self.reads.setdefault(self._key(b), []).append(ev)
        for b in w:
            k = self._key(b)
            self.lastw[k] = ev
            self.reads[k] = []

    def op(self, eng, fn, r=(), w=()):
        self._deps(eng, r, w, same_engine_ok=(eng is self.PE))
        inst = fn(eng)
        sem = self.esem[id(eng)]
        self.ecnt[id(eng)] += 1
        inst.then_inc(sem, 1)
        ev = (sem, self.ecnt[id(eng)])
        self._record(ev, r, w)
        self.n_inst += 1
        return ev

    def dma(self, q, out, in_, r=(), w=(), is_output=False, **kw):
        self._deps(q, r, w)
        import os
        if q is self.POOL and os.environ.get("KB_UNIQUE_POOL_SEMS"):
            self.dsems.append(self.stack0.enter_context(self.nc.semaphore(f"dsx{len(self.dsems)}")))
            self.dcnt.append(0)
            slot = len(self.dsems) - 1
        else:
            slot = self.dnext
            self.dnext = (self.dnext + 1) % self.N_DMA_SEMS
        sem = self.dsems[slot]
        if self.dcnt[slot] > 0:
            self._wait(q, (sem, self.dcnt[slot]))
        inst = out(q) if callable(out) else q.dma_start(out=out, in_=in_, **kw)
        self.dcnt[slot] += 16
        inst.then_inc(sem, 16)
        ev = (sem, self.dcnt[slot])
        self._record(ev, r, w)
        if is_output:
            self.out_events.append(ev)
        self.n_inst += 1
        return ev

    def _finish_dma(self, q, inst, r, w):
        slot = self.dnext
        self.dnext = (self.dnext + 1) % self.N_DMA_SEMS
        sem = self.dsems[slot]
        self.dcnt[slot] += 16
        inst.then_inc(sem, 16)
        ev = (sem, self.dcnt[slot])
        self._record(ev, r, w)
        self.n_inst += 1
        return ev

    def bound_reg(self, val):
        regs = self.__dict__.setdefault("_bregs", {})
        if val not in regs:
            regs[val] = self.nc.gpsimd.to_reg(val)
        return regs[val]

    def finish(self):
        for ev in self.out_events:
            self._wait(self.SP, ev)
        for slot in range(len(self.dsems)):
            if self.dcnt[slot] > 0:
                self._wait(self.SP, (self.dsems[slot], self.dcnt[slot]))
        for e in self.engs:
            if e is not self.SP and self.ecnt[id(e)] > 0:
                self._wait(self.SP, (self.esem[id(e)], self.ecnt[id(e)]))
        self.stack.close()


    def barrier(self):
        evs = []
        for e in self.engs:
            if self.ecnt[id(e)] > 0:
                evs.append((self.esem[id(e)], self.ecnt[id(e)]))
        for slot in range(len(self.dsems)):
            if self.dcnt[slot] > 0:
                evs.append((self.dsems[slot], self.dcnt[slot]))
        for e in self.engs:
            for ev in evs:
                self._wait(e, ev)
        self.lastw.clear()
        self.reads.clear()

    @contextlib.contextmanager
    def scope(self):
        outer = self.stack
        self.stack = contextlib.ExitStack()
        try:
            yield
        finally:
            self.barrier()
            self.stack.close()
            self.stack = outer

    def mm(self, out, lhsT, rhs, start=True, stop=True, r=(), w=()):
        return self.op(self.PE, lambda e: e.matmul(out, lhsT=lhsT, rhs=rhs, start=start, stop=stop), r=r, w=w)

    def act(self, out, in_, func, r=(), w=(), **kw):
        return self.op(self.ACT, lambda e: e.activation(out=out, in_=in_, func=func, **kw), r=r, w=w)

    def tt(self, eng, out, in0, in1, op, r=(), w=()):
        return self.op(eng, lambda e: e.tensor_tensor(out=out, in0=in0, in1=in1, op=op), r=r, w=w)

    def ts(self, eng, out, in0, s1, op0, s2=None, op1=None, r=(), w=(), **kw):
        if op1 is None:
            return self.op(eng, lambda e: e.tensor_scalar(out=out, in0=in0, scalar1=s1, scalar2=None, op0=op0, **kw), r=r, w=w)
        return self.op(eng, lambda e: e.tensor_scalar(out=out, in0=in0, scalar1=s1, scalar2=s2, op0=op0, op1=op1, **kw), r=r, w=w)

    def stt(self, eng, out, in0, scalar, in1, op0, op1, r=(), w=()):
        return self.op(eng, lambda e: e.scalar_tensor_tensor(out=out, in0=in0, scalar=scalar, in1=in1, op0=op0, op1=op1), r=r, w=w)

    def copy(self, eng, out, in_, r=(), w=()):
        if eng is self.ACT:
            return self.op(eng, lambda e: e.copy(out=out, in_=in_), r=r, w=w)
        return self.op(eng, lambda e: e.tensor_copy(out=out, in_=in_), r=r, w=w)

    def memset(self, eng, ap, val, w=()):
        return self.op(eng, lambda e: e.memset(ap, val), w=w)

    def transpose(self, out, in_, ident, r=(), w=()):
        return self.op(self.PE, lambda e: e.transpose(out, in_, ident), r=r, w=w)


class Ring:
    def __init__(self, K, name, shape, dtype, n, psum=False):
        self.bufs = [(K.ps if psum else K.sb)(f"{name}{i}", shape, dtype) for i in range(n)]
        self.i = 0

    def next(self):
        b = self.bufs[self.i % len(self.bufs)]
        self.i += 1
        return b


EPS = 1e-6
NEG = -60000.0
N_TM = 9 * 512
N_FM = 28 * 128
C_Z, C_Q, C_K, C_V, C_G, C_DT = 0, 1024, 1536, 2048, 3072, 4096


class Dims:
    def __init__(self, ntc=2, ntl=64):
        self.ntc, self.ntl = ntc, ntl
        self.nt = ntc + ntl
        self.T = 128 * self.nt
        self.TC = 128 * ntc
        self.TL = 128 * ntl
        self.blocks = [(0, self.TC, 1)] + [(self.TC + i * 512, 512, 0) for i in range(self.TL // 512)]


def make_consts(K):
    c = {}
    def tri(name, pattern, cm, op, base_val, fill):
        t = K.sb(name, [128, 128], F32)
        K.memset(K.POOL, t[:], base_val, w=[t])
        K.op(K.POOL, lambda e: e.affine_select(out=t[:], in_=t[:], pattern=pattern, compare_op=op, fill=fill,
                                               base=0, channel_multiplier=cm), r=[t], w=[t])
        return t
    c["ident"] = tri("ident", [[-1, 128]], 1, ALU.is_equal, 1.0, 0.0)
    c["U0"] = tri("Ufwd", [[1, 128]], -1, ALU.is_ge, 1.0, 0.0)
    c["U1"] = tri("Urev", [[-1, 128]], 1, ALU.is_ge, 1.0, 0.0)
    c["M0"] = tri("Mfwd", [[1, 128]], -1, ALU.is_ge, 0.0, NEG)
    c["M1"] = tri("Mrev", [[-1, 128]], 1, ALU.is_ge, 0.0, NEG)
    ones = K.sb("ones", [128, 128], F32)
    K.memset(K.POOL, ones[:], 1.0, w=[ones])
    c["ones"] = ones
    idb = K.sb("identb", [128, 128], BF16)
    K.copy(K.DVE, idb[:], c["ident"][:], r=[c["ident"]], w=[idb])
    c["identb"] = idb
    for nm in ("U0", "U1", "M0", "M1", "ones"):
        tb = K.sb(nm + "b", [128, 128], BF16)
        K.copy(K.DVE, tb[:], c[nm][:], r=[c[nm]], w=[tb])
        c[nm + "b"] = tb
    return c


def rsqrt_mean(K, out, in_, n, r, w):
    K.act(out, in_, AF.Ln, r=r, w=w, scale=1.0 / n, bias=EPS)
    K.act(out, out, AF.Exp, r=w, w=w, scale=-0.5)


def build_mod(K, nc, D, I, l, modT, cst):
    with K.scope():
        condT = K.sb("condT", [128, 8, 2], F32)
        sc = K.sb("siluc", [128, 8, 2], F32)
        bm = K.sb("bmT", [128, 48], F32)
        K.dma(K.SP, condT[:], I["condT"], w=[condT])
        K.dma(K.SP, bm[:], I[f"b_modT{l}"], w=[bm])
        K.act(sc[:], condT[:], AF.Silu, r=[condT], w=[sc])
        wring = Ring(K, "wm", [128, 8, 768], F32, 2)
        pring = Ring(K, "pmod", [128, 512], F32, 2, psum=True)
        wv = I[f"w_mod{l}"].rearrange("(k p) c -> p k c", p=128)
        for jb in range(8):
            wm = wring.next()
            K.dma(K.SP if jb % 2 == 0 else K.ACT, wm[:], wv[:, :, jb * 768:(jb + 1) * 768], w=[wm])
            for j6 in range(6):
                j = jb * 6 + j6
                ps = pring.next()
                for k in range(8):
                    K.mm(ps[:, 0:2], wm[:, k, j6 * 128:(j6 + 1) * 128], sc[:, k, :], start=(k == 0), stop=(k == 7),
                         r=[wm, sc], w=[ps])
                K.ts(K.DVE, modT[:, j, :], ps[:, 0:2], bm[:, j:j + 1], ALU.add, r=[ps, bm], w=[modT])


def build_norm_mod(K, D, src_aps, normw, modT, row_shift, row_scale, hlT, cst, xsum_out=None):
    with K.scope():
        A = K.sb("A", [128, 8, 2], F32)
        K.ts(K.DVE, A[:], modT[:, row_scale * 8:(row_scale + 1) * 8, :], 1.0, ALU.add, r=[modT], w=[A])
        K.tt(K.DVE, A[:], A[:], normw[:].unsqueeze(2).to_broadcast([128, 8, 2]), ALU.mult, r=[A, normw], w=[A])
        xring = Ring(K, "xb", [128, 8, 512], F32, 2)
        x2ring = Ring(K, "xb2", [128, 8, 512], F32, 1) if len(src_aps) > 1 else None
        sq = K.sb("sq", [128, 8, 512], F32) if x2ring is None else x2ring.bufs[0]
        rstd = K.sb("rstd", [128, 512], F32)
        tring = Ring(K, "tmpn", [128, 512], F32, 2)
        pring = Ring(K, "pn", [128, 512], F32, 2, psum=True)
        for bi, (s0, n, r) in enumerate(D.blocks):
            xb = xring.next()
            K.dma(K.SP, xb[:, :, :n], src_aps[0][:, s0:s0 + n].rearrange("(k p) t -> p k t", p=128), w=[xb])
            for extra in src_aps[1:]:
                x2 = x2ring.next()
                K.dma(K.ACT, x2[:, :, :n], extra[:, s0:s0 + n].rearrange("(k p) t -> p k t", p=128), w=[x2])
                K.tt(K.POOL, xb[:, :, :n], xb[:, :, :n], x2[:, :, :n], ALU.add, r=[xb, x2], w=[xb])
            if xsum_out is not None:
                K.dma(K.SP, xsum_out[:, s0:s0 + n].rearrange("(k p) t -> p k t", p=128), xb[:, :, :n], r=[xb])
            K.act(sq[:, :, :n], xb[:, :, :n], AF.Square, r=[xb], w=[sq])
            ps = pring.next()
            for k in range(8):
                K.mm(ps[:, :n], cst["ones"][:], sq[:, k, :n], start=(k == 0), stop=(k == 7), r=[sq], w=[ps])
            rsqrt_mean(K, rstd[:, :n], ps[:, :n], 1024, r=[ps], w=[rstd])
            for k in range(8):
                tmp = tring.next()
                K.tt(K.DVE, tmp[:, :n], xb[:, k, :n], rstd[:, :n], ALU.mult, r=[xb, rstd], w=[tmp])
                K.act(hlT[:, k, s0:s0 + n], tmp[:, :n], AF.Identity, r=[tmp, A, modT], w=[(id(hlT), bi)],
                      scale=A[:, k, r:r + 1], bias=modT[:, row_shift * 8 + k, r:r + 1])


def build_inproj(K, D, I, l, hlT, S, cst):
    W = I[f"w_core{l}"]
    with K.scope():
        wring = Ring(K, "wtm", [128, 8, 512], BF16, 2)
        pring = Ring(K, "pip", [128, 512], F32, 4, psum=True)
        sring = Ring(K, "stg", [128, 512], F32, 4)
        cnt = 0
        for cb in range(9):
            wb = wring.next()
            K.dma(K.POOL, wb[:], W[:, cb * 512:(cb + 1) * 512].rearrange("(k p) c -> p k c", p=128), w=[wb])
            for tt_ in range(D.nt):
                ps = pring.next()
                for k in range(8):
                    K.mm(ps[:], hlT[:, k, tt_ * 128:(tt_ + 1) * 128], wb[:, k, :], start=(k == 0), stop=(k == 7),
                         r=[wb], w=[ps])
                st = sring.next()
                if cb == 3:
                    K.ts(K.DVE, st[:], ps[:], float(128 ** -0.5), ALU.mult, r=[ps], w=[st])
                elif cnt % 2 == 0:
                    K.copy(K.ACT, st[:], ps[:], r=[ps], w=[st])
                else:
                    K.copy(K.DVE, st[:], ps[:], r=[ps], w=[st])
                cnt += 1
                K.dma(K.SP, S["P_tm"][tt_ * 128:(tt_ + 1) * 128, cb * 512:(cb + 1) * 512], st[:], r=[st])
        wring2 = Ring(K, "wfm", [128, 8, 128], BF16, 2)
        for c in range(28):
            wb = wring2.next()
            K.dma(K.POOL, wb[:], W[:, N_TM + c * 128:N_TM + (c + 1) * 128].rearrange("(k p) c -> p k c", p=128), w=[wb])
            for (s0, n, r) in D.blocks:
                ps = pring.next()
                for k in range(8):
                    K.mm(ps[:, :n], wb[:, k, :], hlT[:, k, s0:s0 + n], start=(k == 0), stop=(k == 7), r=[wb], w=[ps])
                st = sring.next()
                if c >= 12:
                    K.act(st[:, :n], ps[:, :n], AF.Sigmoid, r=[ps], w=[st])
                elif cnt % 2 == 0:
                    K.copy(K.ACT, st[:, :n], ps[:, :n], r=[ps], w=[st])
                else:
                    K.copy(K.DVE, st[:, :n], ps[:, :n], r=[ps], w=[st])
                cnt += 1
                K.dma(K.SP, S["PT_fm"][c * 128:(c + 1) * 128, s0:s0 + n], st[:, :n], r=[st])


def build_conv(K, D, I, l, S, cst):
    with K.scope():
        cw = K.sb("cw", [128, 12, 3], F32)
        cb = K.sb("cb", [128, 12], F32)
        K.dma(K.SP, cw[:], I[f"cw{l}"], w=[cw])
        K.dma(K.SP, cb[:], I[f"cbias{l}"], w=[cb])
        cin_ring = Ring(K, "cin", [128, 514], F32, 3)
        acc_ring = Ring(K, "cacc", [128, 512], F32, 2)
        co_ring = Ring(K, "cout", [128, 512], F32, 3)
        cob_ring = Ring(K, "coutb", [128, 512], BF16, 3)
        xs_stage = Ring(K, "xsst", [128, 4, 1024], F32, 2)
        bm_stage = Ring(K, "bmst", [128, 4, 256], BF16, 2)
        pring = Ring(K, "pcv", [128, 512], F32, 4, psum=True)
        for (s0, n, r) in D.blocks:
            first = (s0 == 0) or (s0 == D.TC)
            last = (s0 + n == D.TC) or (s0 + n == D.T)
            nt4 = n // 128
            xst = xs_stage.next()
            bst = bm_stage.next()
            for c in range(12):
                cin = cin_ring.next()
                lo = s0 - (0 if first else 1)
                hi = s0 + n + (0 if last else 1)
                if first:
                    K.memset(K.POOL, cin[:, 0:1], 0.0, w=[cin])
                if last:
                    K.memset(K.POOL, cin[:, n + 1:n + 2], 0.0, w=[cin])
                K.dma(K.SP if c % 2 == 0 else K.ACT, cin[:, (1 if first else 0):(1 if first else 0) + hi - lo],
                      S["PT_fm"][c * 128:(c + 1) * 128, lo:hi], w=[cin])
                acc = acc_ring.next()
                K.ts(K.DVE, acc[:, :n], cin[:, 0:n], cw[:, c, 0:1], ALU.mult, r=[cin, cw], w=[acc])
                K.stt(K.DVE, acc[:, :n], cin[:, 1:n + 1], cw[:, c, 1:2], acc[:, :n], ALU.mult, ALU.add, r=[cin, acc], w=[acc])
                K.stt(K.DVE, acc[:, :n], cin[:, 2:n + 2], cw[:, c, 2:3], acc[:, :n], ALU.mult, ALU.add, r=[cin, acc], w=[acc])
                if c < 8:
                    co = co_ring.next()
                    K.act(co[:, :n], acc[:, :n], AF.Silu, r=[acc, cb], w=[co], bias=cb[:, c:c + 1])
                    ps = pring.next()
                    for j in range(nt4):
                        K.transpose(ps[:, j * 128:(j + 1) * 128], co[:, j * 128:(j + 1) * 128], cst["ident"][:], r=[co], w=[ps])
                    K.copy(K.ACT if c % 2 == 0 else K.DVE, xst[:, :nt4, c * 128:(c + 1) * 128],
                           ps[:, :n].rearrange("p (j q) -> p j q", q=128), r=[ps], w=[xst])
                else:
                    cob = cob_ring.next()
                    K.act(cob[:, :n], acc[:, :n], AF.Silu, r=[acc, cb], w=[cob], bias=cb[:, c:c + 1])
                    dst = S["BT"] if c < 10 else S["CT"]
                    cc = (c - 8) if c < 10 else (c - 10)
                    K.dma(K.SP, dst[cc * 128:(cc + 1) * 128, s0:s0 + n], cob[:, :n], r=[cob])
                    if c < 10:
                        ps = pring.next()
                        psb = ps[:].bitcast(BF16)
                        for j in range(nt4):
                            K.transpose(psb[:, j * 128:(j + 1) * 128], cob[:, j * 128:(j + 1) * 128], cst["identb"][:], r=[cob], w=[ps])
                        K.copy(K.DVE, bst[:, :nt4, cc * 128:(cc + 1) * 128],
                               psb[:, :n].rearrange("p (j q) -> p j q", q=128), r=[ps], w=[bst])
            K.dma(K.SP, S["XS_tm"][s0:s0 + n, :].rearrange("(j p) c -> p j c", p=128), xst[:, :nt4, :], r=[xst])
            K.dma(K.ACT, S["Bm"][s0:s0 + n, :].rearrange("(j p) c -> p j c", p=128), bst[:, :nt4, :], r=[bst])


def build_rope(K, D, I, S, cst):
    with K.scope():
        qk_ring = Ring(K, "qk", [128, 1024], F32, 2)
        cs_ring = Ring(K, "cossin", [128, 128], F32, 2)
        ro_ring = Ring(K, "ro", [128, 1024], F32, 2)
        t_ring = Ring(K, "rt", [128, 8, 64], F32, 2)
        rb_ring = Ring(K, "rob", [128, 1024], BF16, 2)
        tst = Ring(K, "rtst", [128, 8, 128], BF16, 2)
        pring = Ring(K, "prp", [128, 512], F32, 2, psum=True)
        for t in range(D.nt):
            qk = qk_ring.next()
            K.dma(K.SP, qk[:], S["P_tm"][t * 128:(t + 1) * 128, C_Q:C_Q + 1024], w=[qk])
            rb = rb_ring.next()
            if t >= D.ntc:
                tl = t - D.ntc
                cs = cs_ring.next()
                K.dma(K.ACT, cs[:, 0:64], I["cos"][tl * 128:(tl + 1) * 128, :], w=[cs])
                K.dma(K.ACT, cs[:, 64:128], I["sin"][tl * 128:(tl + 1) * 128, :], w=[cs])
                v = qk[:].rearrange("p (h two d) -> p h two d", two=2, d=64)
                t1, t2 = v[:, :, 0, :], v[:, :, 1, :]
                cosb = cs[:, 0:64].unsqueeze(1).to_broadcast([128, 8, 64])
                sinb = cs[:, 64:128].unsqueeze(1).to_broadcast([128, 8, 64])
                ro = ro_ring.next()
                rv = ro[:].rearrange("p (h two d) -> p h two d", two=2, d=64)
                ta = t_ring.next()
                K.tt(K.DVE, rv[:, :, 0, :], t1, cosb, ALU.mult, r=[qk, cs], w=[ro])
                K.tt(K.POOL, ta[:], t2, sinb, ALU.mult, r=[qk, cs], w=[ta])
                K.tt(K.DVE, rv[:, :, 0, :], rv[:, :, 0, :], ta[:], ALU.subtract, r=[ro, ta], w=[ro])
                tb = t_ring.next()
                K.tt(K.DVE, rv[:, :, 1, :], t1, sinb, ALU.mult, r=[qk, cs], w=[ro])
                K.tt(K.POOL, tb[:], t2, cosb, ALU.mult, r=[qk, cs], w=[tb])
                K.tt(K.DVE, rv[:, :, 1, :], rv[:, :, 1, :], tb[:], ALU.add, r=[ro, tb], w=[ro])
                K.copy(K.ACT, rb[:], ro[:], r=[ro], w=[rb])
            else:
                K.copy(K.ACT, rb[:], qk[:], r=[qk], w=[rb])
            K.dma(K.SP, S["Km_r"][t * 128:(t + 1) * 128, :], rb[:, 512:1024], r=[rb])
            st = tst.next()
            for half in range(2):
                ps = pring.next()
                psb = ps[:].bitcast(BF16)
                for j in range(4):
                    jj = half * 4 + j
                    K.transpose(psb[:, j * 128:(j + 1) * 128], rb[:, jj * 128:(jj + 1) * 128], cst["identb"][:], r=[rb], w=[ps])
                K.copy(K.DVE if half == 0 else K.ACT, st[:, half * 4:(half + 1) * 4, :],
                       psb[:, 0:512].rearrange("p (j q) -> p j q", q=128), r=[ps], w=[st])
            K.dma(K.SP, S["QT_r"][:, t * 128:(t + 1) * 128].rearrange("(j p) t -> p j t", p=128), st[:, 0:4, :], r=[st])
            K.dma(K.ACT, S["KT_r"][:, t * 128:(t + 1) * 128].rearrange("(j p) t -> p j t", p=128), st[:, 4:8, :], r=[st])


def build_scan(K, D, I, l, S, cst):
    with K.scope():
        def bload(name, ap, n):
            t = K.sb(name, [128, n], F32)
            K.dma(K.SP, t[:], ap.partition_broadcast(128), w=[t])
            return t
        dtb = bload("dtb", I[f"dtb{l}"], 32)
        negA = bload("negA", I[f"alog{l}"], 32)
        dsk = bload("dsk", I[f"dsk{l}"], 16)
        snw = bload("snw", I[f"snw{l}"], 1024)
        gnw = bload("gnw", I[f"gnw{l}"], 1024)
        laret = bload("laret", I[f"rdec{l}"], 8)
        K.act(negA[:], negA[:], AF.Exp, r=[negA], w=[negA])
        K.ts(K.DVE, negA[:], negA[:], -1.0, ALU.mult, r=[negA], w=[negA])
        K.act(laret[:], laret[:], AF.Exp, r=[laret], w=[laret], scale=-1.0)
        K.act(laret[:], laret[:], AF.Ln, r=[laret], w=[laret], bias=1.0)
        K.ts(K.DVE, laret[:], laret[:], -1.0, ALU.mult, r=[laret], w=[laret])

        laret_hl = K.sb("laret_hl", [128, 2, 8], BF16)
        K.copy(K.DVE, laret_hl[:, 0, :], laret[:], r=[laret], w=[laret_hl])
        K.tt(K.DVE, laret_hl[:, 1, :], laret[:], laret_hl[:, 0, :], ALU.subtract, r=[laret, laret_hl], w=[laret_hl])
        lab_ring = Ring(K, "lab", [128, 2, 8], BF16, 4)
        qt_ring = Ring(K, "qt", [128, 128], BF16, 4)
        kt_ring = Ring(K, "kt", [128, 128], BF16, 4)
        km_ring = Ring(K, "km", [128, 128], BF16, 4)
        v32_ring = Ring(K, "v32", [128, 512], F32, 4)
        dtr_ring = Ring(K, "dtr", [128, 8], F32, 4)
        sm_ring = Ring(K, "sm", [128, 8, 8], F32, 4)
        vbf_ring = Ring(K, "vbf", [128, 512], BF16, 4)
        vs_ring = Ring(K, "vs", [128, 512], BF16, 3)
        dec_ring = Ring(K, "dec", [128, 128], F32, 4)
        L_ring = Ring(K, "L", [128, 128], BF16, 4)
        y_ring = Ring(K, "y", [128, 512], F32, 3)
        yt_ring = Ring(K, "ytmp", [128, 512], F32, 3)
        yp_ring = Ring(K, "yprev", [128, 512], F32, 2)
        z_ring = Ring(K, "zt", [128, 512], F32, 2)
        ob_ring = Ring(K, "ob", [128, 512], BF16, 2)
        ost_ring = Ring(K, "ost", [128, 4, 128], BF16, 2)
        st_ring = Ring(K, "stat", [128, 4], F32, 3)
        S32 = K.sb("S32", [128, 512], F32)
        Sbf = K.sb("Sbf", [128, 512], BF16)
        LOOK = 2
        pab = Ring(K, "pab", [128, 512], F32, 1, psum=True)
        pc = Ring(K, "pc", [128, 512], F32, 2, psum=True)
        pY = Ring(K, "pY", [128, 512], F32, LOOK + 1, psum=True)
        pYS = Ring(K, "pYS", [128, 512], F32, 1, psum=True)
        pKV = Ring(K, "pKV", [128, 512], F32, 1, psum=True)

        units = [("ssd", g) for g in range(2)] + [("ret", h) for h in range(4)]
        import os
        if os.environ.get("SCAN_UNITS"):
            units = [units[int(i)] for i in os.environ["SCAN_UNITS"].split(",")]
        ndirs = int(os.environ.get("SCAN_DIRS", "2"))
        for kind, u in units:
            H, P = (8, 64) if kind == "ssd" else (1, 256)
            HP = H * P
            for d in range(ndirs):
                order = list(range(D.nt)) if d == 0 else (list(range(D.ntc - 1, -1, -1)) + list(range(D.nt - 1, D.ntc - 1, -1)))
                U, M = cst[f"U{d}b"], cst[f"M{d}b"]
                K.memset(K.POOL, S32[:, :HP], 0.0, w=[S32])
                K.memset(K.POOL, Sbf[:, :HP], 0.0, w=[Sbf])
                def front(c):
                    rows = slice(c * 128, (c + 1) * 128)
                    qt, kt, km, v32 = qt_ring.next(), kt_ring.next(), km_ring.next(), v32_ring.next()
                    sm = sm_ring.next()
                    vbf = vbf_ring.next()
                    if kind == "ssd":
                        K.dma(K.SP, qt[:], S["CT"][u * 128:(u + 1) * 128, rows], w=[qt])
                        K.dma(K.SP, kt[:], S["BT"][u * 128:(u + 1) * 128, rows], w=[kt])
                        K.dma(K.SP, km[:], S["Bm"][rows, u * 128:(u + 1) * 128], w=[km])
                        K.dma(K.SP, v32[:, :HP], S["XS_tm"][rows, u * 512:(u + 1) * 512], w=[v32])
                        dtr = dtr_ring.next()
                        K.dma(K.SP, dtr[:], S["P_tm"][rows, C_DT + u * 8:C_DT + (u + 1) * 8], w=[dtr])
                        hs = slice(d * 16 + u * 8, d * 16 + u * 8 + 8)
                        dt_ = sm[:, 0, :]
                        K.tt(K.DVE, dt_, dtr[:], dtb[:, hs], ALU.add, r=[dtr, dtb], w=[(id(sm), 0)])
                        K.act(dt_, dt_, AF.Exp, r=[(id(sm), 0)], w=[(id(sm), 0)])
                        K.act(dt_, dt_, AF.Ln, r=[(id(sm), 0)], w=[(id(sm), 0)], bias=1.0)
                        la = sm[:, 1, :]
                        K.tt(K.DVE, la, dt_, negA[:, hs], ALU.mult, r=[(id(sm), 0), negA], w=[(id(sm), 1)])
                        K.tt(K.DVE, vbf[:, :HP].rearrange("p (h q) -> p h q", h=8), v32[:, :HP].rearrange("p (h q) -> p h q", h=8),
                             dt_.unsqueeze(2).to_broadcast([128, 8, 64]), ALU.mult, r=[v32, (id(sm), 0)], w=[vbf])
                        la_key = (id(sm), 1)
                        lab = lab_ring.next()
                        K.copy(K.DVE, lab[:, 0, :], la, r=[la_key], w=[lab])
                        K.tt(K.DVE, lab[:, 1, :], la, lab[:, 0, :], ALU.subtract, r=[la_key, lab], w=[lab])
                        la_hi, la_lo, lab_key = lab[:, 0, :], lab[:, 1, :], lab
                    else:
                        K.dma(K.SP, qt[:], S["QT_r"][u * 128:(u + 1) * 128, rows], w=[qt])
                        K.dma(K.SP, kt[:], S["KT_r"][u * 128:(u + 1) * 128, rows], w=[kt])
                        K.dma(K.SP, km[:], S["Km_r"][rows, u * 128:(u + 1) * 128], w=[km])
                        K.dma(K.SP, v32[:, :HP], S["P_tm"][rows, C_V + u * 256:C_V + (u + 1) * 256], w=[v32])
                        K.copy(K.ACT, vbf[:, :HP], v32[:, :HP], r=[v32], w=[vbf])
                        la = laret[:, d * 4 + u:d * 4 + u + 1]
                        la_key = laret
                        la_hi = laret_hl[:, 0, d * 4 + u:d * 4 + u + 1]
                        la_lo = laret_hl[:, 1, d * 4 + u:d * 4 + u + 1]
                        lab_key = laret_hl
                    pa_ = pab.next()
                    K.mm(pa_[:, 0:H], U[:], la_hi, start=True, stop=False, r=[lab_key], w=[pa_])
                    K.mm(pa_[:, 0:H], U[:], la_lo, start=False, stop=True, r=[lab_key], w=[pa_])
                    K.mm(pa_[:, 64:64 + H], cst["onesb"][:], la_hi, start=True, stop=False, r=[lab_key], w=[pa_])
                    K.mm(pa_[:, 64:64 + H], cst["onesb"][:], la_lo, start=False, stop=True, r=[lab_key], w=[pa_])
                    negcs, ecs, wdec, elast = sm[:, 2, :H], sm[:, 3, :H], sm[:, 4, :H], sm[:, 5, :H]
                    K.ts(K.DVE, negcs, pa_[:, 0:H], -1.0, ALU.mult, r=[pa_], w=[(id(sm), 2)])
                    K.act(ecs, pa_[:, 0:H], AF.Exp, r=[pa_], w=[(id(sm), 3)])
                    K.tt(K.DVE, wdec, pa_[:, 64:64 + H], negcs, ALU.add, r=[pa_, (id(sm), 2)], w=[(id(sm), 4)])
                    K.act(wdec, wdec, AF.Exp, r=[(id(sm), 4)], w=[(id(sm), 4)])
                    K.act(elast, pa_[:, 64:64 + H], AF.Exp, r=[pa_], w=[(id(sm), 5)])
                    pb_ = pa_
                    K.mm(pb_[:, 128:256], kt[:], qt[:], r=[kt, qt], w=[pb_])
                    pY_ = pY.next()
                    sl = slice(0, 128)

                    def bc(h):
                        pcx = pc.next()
                        K.mm(pcx[:, sl], la_hi[:, h:h + 1].to_broadcast([128, 128]), U[:], start=True, stop=False, r=[lab_key], w=[pcx])
                        K.mm(pcx[:, sl], la_lo[:, h:h + 1].to_broadcast([128, 128]), U[:], start=False, stop=False, r=[lab_key], w=[pcx])
                        K.mm(pcx[:, sl], cst["identb"][:], M[:], start=False, stop=True, w=[pcx])
                        return pcx
                    pcs = {0: bc(0)}
                    for h in range(H):
                        if h + 1 < H:
                            pcs[h + 1] = bc(h + 1)
                        pc_ = pcs.pop(h)
                        ck = pc_
                        dec = dec_ring.next()
                        K.act(dec[:], pc_[:, sl], AF.Exp, r=[ck, (id(sm), 2)], w=[dec], bias=negcs[:, h:h + 1])
                        L = L_ring.next()
                        K.tt(K.DVE, L[:], pb_[:, 128:256], dec[:], ALU.mult, r=[pb_, dec], w=[L])
                        K.mm(pY_[:, h * P:(h + 1) * P], L[:], vbf[:, h * P:(h + 1) * P], r=[L, vbf], w=[pY_])
                    return dict(c=c, rows=rows, qt=qt, km=km, v32=v32, sm=sm, vbf=vbf, pY_=pY_)

                def back(ctx):
                    c, rows, qt, km, v32, sm, vbf, pY_ = (ctx[k] for k in ('c', 'rows', 'qt', 'km', 'v32', 'sm', 'vbf', 'pY_'))
                    ecs, wdec, elast = sm[:, 3, :H], sm[:, 4, :H], sm[:, 5, :H]
                    pYS_ = pYS.next()
                    K.mm(pYS_[:, :HP], qt[:], Sbf[:, :HP], r=[qt, Sbf], w=[pYS_])
                    ytmp = yt_ring.next()
                    y = y_ring.next()
                    K.tt(K.DVE, ytmp[:, :HP].rearrange("p (h q) -> p h q", h=H), pYS_[:, :HP].rearrange("p (h q) -> p h q", h=H),
                         ecs.unsqueeze(2).to_broadcast([128, H, P]), ALU.mult, r=[pYS_, (id(sm), 3)], w=[ytmp])
                    K.tt(K.DVE, y[:, :HP], pY_[:, :HP], ytmp[:, :HP], ALU.add, r=[pY_, ytmp], w=[y])
                    vs = vs_ring.next()
                    K.tt(K.POOL, vs[:, :HP].rearrange("p (h q) -> p h q", h=H), vbf[:, :HP].rearrange("p (h q) -> p h q", h=H),
                         wdec.unsqueeze(2).to_broadcast([128, H, P]), ALU.mult, r=[vbf, (id(sm), 4)], w=[vs])
                    pKV_ = pKV.next()
                    K.mm(pKV_[:, :HP], km[:], vs[:, :HP], r=[km, vs], w=[pKV_])
                    K.tt(K.DVE, S32[:, :HP].rearrange("p (h q) -> p h q", h=H), S32[:, :HP].rearrange("p (h q) -> p h q", h=H),
                         elast.unsqueeze(2).to_broadcast([128, H, P]), ALU.mult, r=[S32, (id(sm), 5), pYS_], w=[S32])
                    K.tt(K.DVE, S32[:, :HP], pKV_[:, :HP], S32[:, :HP], ALU.add, r=[pKV_, S32], w=[S32])
                    K.copy(K.ACT, Sbf[:, :HP], S32[:, :HP], r=[S32], w=[Sbf])
                    yacc = S["YS"] if kind == "ssd" else S["YR"]
                    ycols = slice(u * HP, (u + 1) * HP)
                    ykey = ("yacc", kind, u, c)
                    if d == 0:
                        if kind == "ssd":
                            K.tt(K.POOL, ytmp[:, :HP].rearrange("p (h q) -> p h q", h=H), v32[:, :HP].rearrange("p (h q) -> p h q", h=H),
                                 dsk[:, u * 8:(u + 1) * 8].unsqueeze(2).to_broadcast([128, H, P]), ALU.mult, r=[v32, dsk, y], w=[ytmp])
                            K.tt(K.DVE, y[:, :HP], y[:, :HP], ytmp[:, :HP], ALU.add, r=[y, ytmp], w=[y])
                        K.dma(K.SP, yacc[rows, ycols], y[:, :HP], r=[y], w=[ykey])
                        return
                    yp = yp_ring.next()
                    K.dma(K.SP, yp[:, :HP], yacc[rows, ycols], r=[ykey], w=[yp])
                    K.tt(K.DVE, y[:, :HP], y[:, :HP], yp[:, :HP], ALU.add, r=[y, yp], w=[y])
                    zt = z_ring.next()
                    ob = ob_ring.next()
                    st = st_ring.next()
                    K.memset(K.POOL, st[:], 0.0, w=[st])
                    if kind == "ssd":
                        K.dma(K.SP, zt[:, :HP], S["P_tm"][rows, C_Z + u * 512:C_Z + (u + 1) * 512], w=[zt])
                        K.act(zt[:, :HP], zt[:, :HP], AF.Silu, r=[zt], w=[zt])
                        K.tt(K.DVE, y[:, :HP], y[:, :HP], zt[:, :HP], ALU.mult, r=[y, zt], w=[y])
                        K.act(ytmp[:, :HP], y[:, :HP], AF.Square, r=[y], w=[ytmp, st], accum_out=st[:, 0:1])
                        rsqrt_mean(K, st[:, 1:2], st[:, 0:1], HP, r=[st], w=[st])
                        K.stt(K.DVE, ob[:, :HP], y[:, :HP], st[:, 1:2], snw[:, ycols], ALU.mult, ALU.mult, r=[y, st, snw], w=[ob])
                        dstT = S["SOT"]
                    else:
                        K.dma(K.SP, zt[:, :HP], S["P_tm"][rows, C_G + u * 256:C_G + (u + 1) * 256], w=[zt])
                        K.act(zt[:, :HP], zt[:, :HP], AF.Silu, r=[zt], w=[zt])
                        K.op(K.DVE, lambda e: e.reduce_sum(out=st[:, 2:3], in_=y[:, :HP], axis=AX.X), r=[y], w=[st])
                        K.ts(K.DVE, st[:, 2:3], st[:, 2:3], 1.0 / HP, ALU.mult, r=[st], w=[st])
                        K.ts(K.DVE, y[:, :HP], y[:, :HP], st[:, 2:3], ALU.subtract, r=[y, st], w=[y])
                        K.act(ytmp[:, :HP], y[:, :HP], AF.Square, r=[y], w=[ytmp, st], accum_out=st[:, 0:1])
                        rsqrt_mean(K, st[:, 1:2], st[:, 0:1], HP, r=[st], w=[st])
                        K.stt(K.DVE, y[:, :HP], y[:, :HP], st[:, 1:2], gnw[:, ycols], ALU.mult, ALU.mult, r=[y, st, gnw], w=[y])
                        K.tt(K.DVE, ob[:, :HP], y[:, :HP], zt[:, :HP], ALU.mult, r=[y, zt], w=[ob])
                        dstT = S["ROT"]
                    nj = HP // 128
                    pT_ = pab.next()
                    pTb = pT_[:, 256:512].bitcast(BF16)
                    for j in range(nj):
                        K.transpose(pTb[:, j * 128:(j + 1) * 128], ob[:, j * 128:(j + 1) * 128], cst["identb"][:], r=[ob], w=[pT_])
                    ost = ost_ring.next()
                    K.copy(K.ACT, ost[:, :nj, :], pTb[:, :nj * 128].rearrange("p (j q) -> p j q", q=128), r=[pT_], w=[ost])
                    K.dma(K.SP, dstT[u * HP:(u + 1) * HP, rows].rearrange("(j p) t -> p j t", p=128), ost[:, :nj, :], r=[ost])

                pend = []
                for c in order:
                    pend.append(front(c))
                    if len(pend) > LOOK:
                        back(pend.pop(0))
                while pend:
                    back(pend.pop(0))


def build_outproj(K, D, I, l, S, cst, modT, deltaT):
    with K.scope():
        ws = {}
        for nm in ("wsso", "wro", "wo"):
            ws[nm] = K.sb(nm, [128, 8, 1024], BF16)
            K.dma(K.POOL, ws[nm][:], I[f"{nm}{l}"].rearrange("(k p) c -> p k c", p=128), w=[ws[nm]])
        sot_ring = Ring(K, "sot", [128, 8, 512], BF16, 2)
        rot_ring = Ring(K, "rot", [128, 8, 512], BF16, 2)
        mT_ring = Ring(K, "mT", [128, 8, 512], BF16, 2)
        sg_ring = Ring(K, "sg", [128, 2, 512], F32, 3)
        m_ring = Ring(K, "mtmp", [128, 2, 512], F32, 3)
        st_ring = Ring(K, "ostg", [128, 512], F32, 3)
        pring = Ring(K, "pop", [128, 512], F32, 6, psum=True)
        for (s0, n, r) in D.blocks:
            sot, rot, mT = sot_ring.next(), rot_ring.next(), mT_ring.next()
            K.dma(K.SP, sot[:, :, :n], S["SOT"][:, s0:s0 + n].rearrange("(k p) t -> p k t", p=128), w=[sot])
            K.dma(K.ACT, rot[:, :, :n], S["ROT"][:, s0:s0 + n].rearrange("(k p) t -> p k t", p=128), w=[rot])
            for dc in range(8):
                ps1, ps2 = pring.next(), pring.next()
                for k in range(8):
                    K.mm(ps1[:, :n], ws["wsso"][:, k, dc * 128:(dc + 1) * 128], sot[:, k, :n], start=(k == 0), stop=(k == 7),
                         r=[ws["wsso"], sot], w=[ps1])
                for k in range(8):
                    K.mm(ps2[:, :n], ws["wro"][:, k, dc * 128:(dc + 1) * 128], rot[:, k, :n], start=(k == 0), stop=(k == 7),
                         r=[ws["wro"], rot], w=[ps2])
                sg = sg_ring.next()
                K.dma(K.SP, sg[:, 0, :n], S["PT_fm"][(12 + dc) * 128:(13 + dc) * 128, s0:s0 + n], w=[sg])
                K.dma(K.ACT, sg[:, 1, :n], S["PT_fm"][(20 + dc) * 128:(21 + dc) * 128, s0:s0 + n], w=[sg])
                mt = m_ring.next()
                K.tt(K.DVE, mt[:, 0, :n], ps1[:, :n], sg[:, 0, :n], ALU.mult, r=[ps1, sg], w=[(id(mt), 0)])
                K.tt(K.DVE, mt[:, 1, :n], ps2[:, :n], sg[:, 1, :n], ALU.mult, r=[ps2, sg], w=[(id(mt), 1)])
                K.tt(K.POOL, mT[:, dc, :n], mt[:, 0, :n], mt[:, 1, :n], ALU.add, r=[(id(mt), 0), (id(mt), 1)], w=[(id(mT), dc)])
            for dc in range(8):
                ps = pring.next()
                for k in range(8):
                    K.mm(ps[:, :n], ws["wo"][:, k, dc * 128:(dc + 1) * 128], mT[:, k, :n], start=(k == 0), stop=(k == 7),
                         r=[ws["wo"]] + [(id(mT), kk) for kk in range(8)], w=[ps])
                st = st_ring.next()
                K.ts(K.DVE, st[:, :n], ps[:, :n], modT[:, 16 + dc, r:r + 1], ALU.mult, r=[ps, modT], w=[st])
                K.dma(K.SP, deltaT[dc * 128:(dc + 1) * 128, s0:s0 + n], st[:, :n], r=[st], w=[("delta", dc, s0)])


def alloc_mixer_scratch(nc, D, tag=""):
    T = D.T
    def dt_(name, shape, dtype):
        return nc.dram_tensor(name + tag, list(shape), dtype, kind="Internal").ap()
    return {
        "P_tm": dt_("P_tm", [T, N_TM], F32), "PT_fm": dt_("PT_fm", [N_FM, T], F32),
        "XS_tm": dt_("XS_tm", [T, 1024], F32), "BT": dt_("BT", [256, T], BF16), "CT": dt_("CT", [256, T], BF16),
        "Bm": dt_("Bm", [T, 256], BF16), "Km_r": dt_("Km_r", [T, 512], BF16),
        "QT_r": dt_("QT_r", [512, T], BF16), "KT_r": dt_("KT_r", [512, T], BF16),
        "YS": dt_("YS", [T, 1024], F32), "YR": dt_("YR", [T, 1024], F32),
        "SOT": dt_("SOT", [1024, T], BF16), "ROT": dt_("ROT", [1024, T], BF16),
    }


def mixer_input_specs(D, l):
    return {
        f"w_mod{l}": ([1024, 6144], F32), f"b_modT{l}": ([128, 48], F32), f"nmw{l}": ([128, 8], F32), f"nfw{l}": ([128, 8], F32),
        f"w_core{l}": ([1024, N_TM + N_FM], F32), f"cw{l}": ([128, 12, 3], F32), f"cbias{l}": ([128, 12], F32),
        f"dtb{l}": ([1, 32], F32), f"alog{l}": ([1, 32], F32), f"dsk{l}": ([1, 16], F32), f"snw{l}": ([1, 1024], F32),
        f"rdec{l}": ([1, 8], F32), f"gnw{l}": ([1, 1024], F32),
        f"wsso{l}": ([1024, 1024], F32), f"wro{l}": ([1024, 1024], F32), f"wo{l}": ([1024, 1024], F32),
    }


def build_mixer_layer(K, nc, D, I, l, S, cst, modT, x_srcs, deltaT, xsum_out=None, nphase=99):
    build_mod(K, nc, D, I, l, modT, cst)
    if nphase < 2:
        return
    with K.scope():
        hlT = K.sb("hlT", [128, 8, D.T], BF16)
        nmw = K.sb("nmw", [128, 8], F32)
        K.dma(K.SP, nmw[:], I[f"nmw{l}"], w=[nmw])
        build_norm_mod(K, D, x_srcs, nmw, modT, 0, 1, hlT, cst, xsum_out=xsum_out)
        if nphase >= 3:
            build_inproj(K, D, I, l, hlT, S, cst)
    if nphase >= 4:
        build_conv(K, D, I, l, S, cst)
    if nphase >= 5:
        build_rope(K, D, I, S, cst)
    if nphase >= 6:
        build_scan(K, D, I, l, S, cst)
    if nphase >= 7:
        build_outproj(K, D, I, l, S, cst, modT, deltaT)


def rope_tables(TL, grid_w=64):
    rows = TL // grid_w
    r, col = np.meshgrid(np.arange(rows), np.arange(grid_w), indexing="ij")
    n_freq = 32
    inv = (np.float32(10000.0) ** (-np.arange(n_freq, dtype=np.float32) / np.float32(n_freq))).astype(np.float32)
    ang = np.concatenate([r.reshape(-1, 1).astype(np.float32) * inv, col.reshape(-1, 1).astype(np.float32) * inv], axis=-1)
    return np.cos(ang).astype(np.float32), np.sin(ang).astype(np.float32)


def fm(v, nchunk):
    return np.ascontiguousarray(np.asarray(v, np.float32).reshape(nchunk, 128).T)


def prep_mixer_inputs(inp, l, s):
    w_in = np.asarray(inp["w_in"][l], np.float32)
    o_z, o_x, o_dt, o_q, o_k, o_v, o_g, o_gs, o_gr = 0, 2048, 5120, 5152, 6176, 7200, 9248, 11296, 12320
    cols_tm = [w_in[:, o_z + s * 1024:o_z + (s + 1) * 1024], w_in[:, o_q + s * 512:o_q + (s + 1) * 512],
               w_in[:, o_k + s * 512:o_k + (s + 1) * 512], w_in[:, o_v + s * 1024:o_v + (s + 1) * 1024],
               w_in[:, o_g + s * 1024:o_g + (s + 1) * 1024], w_in[:, o_dt + s * 16:o_dt + (s + 1) * 16],
               np.zeros((1024, 496), np.float32)]
    xs_c = np.arange(s * 1024, (s + 1) * 1024)
    b_c = 2048 + np.arange(s * 256, (s + 1) * 256)
    c_c = 2560 + np.arange(s * 256, (s + 1) * 256)
    xbc_ch = np.concatenate([xs_c, b_c, c_c])
    cols_fm = [w_in[:, o_x + xbc_ch], w_in[:, o_gs:o_gs + 1024], w_in[:, o_gr:o_gr + 1024]]
    w_core = np.ascontiguousarray(np.concatenate(cols_tm + cols_fm, axis=1))
    cw = np.asarray(inp["conv_w"][l], np.float32)[:, xbc_ch]
    cwT = np.ascontiguousarray(cw.reshape(3, 12, 128).transpose(2, 1, 0))
    cbT = fm(np.asarray(inp["conv_b"][l], np.float32)[xbc_ch], 12)
    hsl = slice(s * 16, (s + 1) * 16)
    out = {
        f"w_mod{l}": np.ascontiguousarray(inp["w_mod"][l], np.float32), f"b_modT{l}": fm(inp["b_mod"][l], 48),
        f"nmw{l}": fm(inp["norm_mix_w"][l], 8), f"nfw{l}": fm(inp["norm_ffn_w"][l], 8),
        f"w_core{l}": w_core, f"cw{l}": cwT, f"cbias{l}": cbT,
        f"dtb{l}": np.ascontiguousarray(np.asarray(inp["ssd_dt_bias"][l], np.float32)[:, hsl].reshape(1, 32)),
        f"alog{l}": np.ascontiguousarray(np.asarray(inp["ssd_a_log"][l], np.float32)[:, hsl].reshape(1, 32)),
        f"dsk{l}": np.ascontiguousarray(np.asarray(inp["ssd_d"][l], np.float32)[hsl].reshape(1, 16)),
        f"snw{l}": np.ascontiguousarray(np.asarray(inp["ssd_norm_w"][l], np.float32)[s * 1024:(s + 1) * 1024].reshape(1, 1024)),
        f"rdec{l}": np.ascontiguousarray(np.asarray(inp["ret_decay"][l], np.float32)[:, s * 4:(s + 1) * 4].reshape(1, 8)),
        f"gnw{l}": np.ascontiguousarray(np.asarray(inp["ret_gn_w"][l], np.float32)[s * 1024:(s + 1) * 1024].reshape(1, 1024)),
        f"wsso{l}": np.ascontiguousarray(np.asarray(inp["w_ssd_o"][l], np.float32)[s * 1024:(s + 1) * 1024]),
        f"wro{l}": np.ascontiguousarray(np.asarray(inp["w_ret_o"][l], np.float32)[s * 1024:(s + 1) * 1024]),
        f"wo{l}": np.ascontiguousarray(inp["w_o"][l], np.float32),
    }
    return out


def prep_common_inputs(inp, b, D):
    xfull = np.concatenate([np.asarray(inp["ctx"][b], np.float32), np.asarray(inp["x"][b], np.float32)], axis=0)
    cond = np.stack([np.asarray(inp["c"][b], np.float32), np.asarray(inp["c_ctx"], np.float32)], axis=-1)
    cos, sin = rope_tables(D.TL)
    return {"xT": np.ascontiguousarray(xfull.T), "condT": np.ascontiguousarray(cond.reshape(8, 128, 2).transpose(1, 0, 2)),
            "cos": cos, "sin": sin}


ROWW = 1024 + 32
BIGPOS = 1.0e6


def alloc_ffn_scratch(nc, D, n_exp, tag=""):
    def dt_(name, shape, dtype):
        return nc.dram_tensor(name + tag, list(shape), dtype, kind="Internal").ap()
    capl, capc = D.TL // 8, D.TC // 8
    return {"HL2": dt_("HL2", [D.T, ROWW], I16), "AFFT": dt_("AFFT", [16, D.T], F32),
            "XINL": [dt_(f"XINL{i}", [capl, ROWW], I16) for i in range(n_exp)],
            "XINC": [dt_(f"XINC{i}", [capc, ROWW], I16) for i in range(n_exp)],
            "OUTL": [dt_(f"OUTL{i}", [capl, 1024], F32) for i in range(n_exp)],
            "OUTC": [dt_(f"OUTC{i}", [capc, 1024], F32) for i in range(n_exp)]}


def ffn_input_specs(l, n_exp):
    return {f"w_router{l}": ([1024, 16], F32), f"wg{l}": ([n_exp, 1024, 2048], F32), f"wu{l}": ([n_exp, 1024, 2048], F32),
            f"wd{l}": ([n_exp, 2048, 1024], F32)}


def build_ffn_layer(K, nc, D, I, l, F, cst, modT, x_srcs, X1T, outT, exp_ids, do_ctx, out_scale):
    n_exp = len(exp_ids)
    sets = [("L", D.ntc, D.nt, D.TL // 8)] + ([("C", 0, D.ntc, D.TC // 8)] if do_ctx else [])
    with K.scope():
        POS = K.sb("POS", [128, D.nt, 16], I32)
        with K.scope():
            hlT = K.sb("hl2T", [128, 8, D.T], BF16)
            nfw = K.sb("nfw", [128, 8], F32)
            K.dma(K.SP, nfw[:], I[f"nfw{l}"], w=[nfw])
            build_norm_mod(K, D, x_srcs, nfw, modT, 3, 4, hlT, cst, xsum_out=X1T)
            wr32 = K.sb("wr32", [128, 8, 16], F32)
            K.dma(K.SP, wr32[:], I[f"w_router{l}"].rearrange("(k p) e -> p k e", p=128), w=[wr32])
            wrb = K.sb("wrb", [128, 8, 16], BF16)
            K.copy(K.DVE, wrb[:], wr32[:], r=[wr32], w=[wrb])
            aff_ring = Ring(K, "affb", [16, 512], F32, 2)
            e_ring = Ring(K, "eexp", [16, 512], F32, 2)
            rs_ring = Ring(K, "rsum", [16, 512], F32, 2)
            row_ring = Ring(K, "row", [128, ROWW], I16, 3)
            pl = Ring(K, "plog", [128, 512], F32, 2, psum=True)
            pt = Ring(K, "ptr", [128, 512], F32, 3, psum=True)
            for (s0, n, r) in D.blocks:
                ps = pl.next()
                for k in range(8):
                    K.mm(ps[0:16, :n], wrb[:, k, :], hlT[:, k, s0:s0 + n], start=(k == 0), stop=(k == 7), r=[wrb], w=[ps])
                ee = e_ring.next()
                K.act(ee[:, :n], ps[0:16, :n], AF.Exp, r=[ps], w=[ee])
                ps2 = pl.next()
                K.mm(ps2[0:16, :n], cst["ones"][0:16, 0:16], ee[:, :n], r=[ee], w=[ps2])
                rs = rs_ring.next()
                K.op(K.DVE, lambda e_: e_.reciprocal(out=rs[:, :n], in_=ps2[0:16, :n]), r=[ps2], w=[rs])
                affb = aff_ring.next()
                K.tt(K.DVE, affb[:, :n], ee[:, :n], rs[:, :n], ALU.mult, r=[ee, rs], w=[affb])
                K.dma(K.ACT, F["AFFT"][:, s0:s0 + n], affb[:, :n], r=[affb])
                for j in range(n // 128):
                    t0 = s0 + j * 128
                    row = row_ring.next()
                    for half in range(2):
                        pp = pt.next()
                        ppb = pp[:].bitcast(BF16)
                        for kk in range(4):
                            k = half * 4 + kk
                            K.transpose(ppb[:, kk * 128:(kk + 1) * 128], hlT[:, k, t0:t0 + 128], cst["identb"][:], w=[pp])
                        K.copy(K.ACT if half == 0 else K.DVE, row[:, half * 512:(half + 1) * 512].bitcast(BF16), ppb[:, 0:512], r=[pp], w=[(id(row), half)])
                    pa = pt.next()
                    K.transpose(pa[:, 0:16], affb[:, j * 128:(j + 1) * 128], cst["ident"][0:16, 0:16], r=[affb], w=[pa])
                    K.copy(K.DVE, row[:, 1024:ROWW].bitcast(F32), pa[:, 0:16], r=[pa], w=[(id(row), 2)])
                    K.dma(K.SP, F["HL2"][t0:t0 + 128, :], row[:], r=[(id(row), 0), (id(row), 1), (id(row), 2)])
        with K.scope():
            affT = K.sb("affT", [16, D.T], F32)
            K.dma(K.SP, affT[:], F["AFFT"], w=[affT])
            bs = K.sb("bis", [16, 8], F32)
            junk = K.sb("junk", [16, D.TL], F32)
            msk_ring = Ring(K, "msk", [16, 128], F32, 2)
            mtm_ring = Ring(K, "mtm", [128, 16], F32, 2)
            accm = K.sb("accm", [128, 16], F32)
            pf_ring = Ring(K, "posf", [128, 16], F32, 2)
            pp_ring = Ring(K, "ppos", [128, 512], F32, 2, psum=True)
            pm_ring = Ring(K, "pmsk", [128, 512], F32, 2, psum=True)
            K.memset(K.DVE, POS[:], 0, w=[POS])
            for (sname, ta, tb, cap) in sets:
                c0, c1 = ta * 128, tb * 128
                K.memset(K.DVE, bs[:, 0:1], 0.0, w=[bs])
                K.memset(K.DVE, bs[:, 1:2], 1.0, w=[bs])
                for it in range(30):
                    K.tt(K.DVE, bs[:, 2:3], bs[:, 0:1], bs[:, 1:2], ALU.add, r=[bs], w=[bs])
                    K.ts(K.DVE, bs[:, 2:3], bs[:, 2:3], 0.5, ALU.mult, r=[bs], w=[bs])
                    K.memset(K.DVE, bs[:, 3:4], 0.0, w=[bs])
                    K.ts(K.DVE, junk[:, :c1 - c0], affT[:, c0:c1], bs[:, 2:3], ALU.is_ge, 0.0, ALU.add, r=[bs, affT], w=[bs, junk],
                         accum_out=bs[:, 3:4])
                    K.ts(K.DVE, bs[:, 4:5], bs[:, 3:4], float(cap), ALU.is_ge, r=[bs], w=[bs])
                    K.tt(K.DVE, bs[:, 5:6], bs[:, 2:3], bs[:, 0:1], ALU.subtract, r=[bs], w=[bs])
                    K.stt(K.DVE, bs[:, 0:1], bs[:, 5:6], bs[:, 4:5], bs[:, 0:1], ALU.mult, ALU.add, r=[bs], w=[bs])
                    K.tt(K.DVE, bs[:, 5:6], bs[:, 1:2], bs[:, 2:3], ALU.subtract, r=[bs], w=[bs])
                    K.stt(K.DVE, bs[:, 1:2], bs[:, 5:6], bs[:, 4:5], bs[:, 2:3], ALU.mult, ALU.add, r=[bs], w=[bs])
                K.memset(K.POOL, accm[:], 0.0, w=[accm])
                for t in range(ta, tb):
                    mk = msk_ring.next()
                    K.ts(K.DVE, mk[:], affT[:, t * 128:(t + 1) * 128], bs[:, 0:1], ALU.is_ge, r=[bs, affT], w=[mk])
                    pm = pm_ring.next()
                    K.transpose(pm[:, 0:16], mk[:], cst["ident"][0:16, 0:16], r=[mk], w=[pm])
                    mtm = mtm_ring.next()
                    K.copy(K.ACT, mtm[:], pm[:, 0:16], r=[pm], w=[mtm])
                    pp = pp_ring.next()
                    K.mm(pp[:, 0:16], cst["U0"][:], mtm[:], start=True, stop=False, r=[mtm], w=[pp])
                    K.mm(pp[:, 0:16], cst["ones"][:], accm[:], start=False, stop=True, r=[accm], w=[pp])
                    pf = pf_ring.next()
                    K.ts(K.DVE, pf[:], pp[:, 0:16], -1.0 - BIGPOS, ALU.add, r=[pp], w=[pf])
                    K.tt(K.DVE, pf[:], pf[:], mtm[:], ALU.mult, r=[pf, mtm], w=[pf])
                    K.ts(K.DVE, pf[:], pf[:], BIGPOS, ALU.add, r=[pf], w=[pf])
                    K.copy(K.DVE, POS[:, t, :], pf[:], r=[pf], w=[("POS", t)])
                    K.tt(K.POOL, accm[:], accm[:], mtm[:], ALU.add, r=[accm, mtm], w=[accm])
        with K.scope():
            row_ring = Ring(K, "drow", [128, ROWW], I16, 3)
            for (sname, ta, tb, cap) in sets:
                XIN = F["XINL"] if sname == "L" else F["XINC"]
                for t in range(ta, tb):
                    row = row_ring.next()
                    K.dma(K.SP, row[:], F["HL2"][t * 128:(t + 1) * 128, :], w=[row])
                    for ei, e in enumerate(exp_ids):
                        K.dma(K.POOL, lambda q, ei=ei, e=e, t=t, row=row, XIN=XIN, cap=cap: q.indirect_dma_start(
                            out=XIN[ei], out_offset=bass.IndirectOffsetOnAxis(ap=POS[:, t, e:e + 1], axis=0),
                            in_=row[:], in_offset=None, bounds_check=K.bound_reg(cap - 1), oob_is_err=False),
                            None, r=[row, ("POS", t)], w=[("XIN", sname, ei)])
        with K.scope():
            wring = Ring(K, "wexp", [128, 16384], BF16, 3)
            xin_ring = Ring(K, "xin", [128, ROWW], I16, 3)
            xinT = K.sb("xinT", [128, 8, 1024], BF16)
            hidT = K.sb("hidT", [128, 16, 1024], BF16)
            gs_ring = Ring(K, "gsel", [128, 8, 16], F32, 2)
            sg_ring = Ring(K, "sgate", [128, 512], F32, 2)
            o_ring = Ring(K, "oexp", [128, 1024], F32, 2)
            pg = Ring(K, "pg", [128, 512], F32, 2, psum=True)
            pu = Ring(K, "pu", [128, 512], F32, 2, psum=True)
            po = Ring(K, "po", [128, 512], F32, 2, psum=True)
            px = Ring(K, "px", [128, 512], F32, 2, psum=True)
            for ei, e in enumerate(exp_ids):
                wg_, wu_, wd_ = wring.next(), wring.next(), wring.next()
                wg = wg_[:].rearrange("p (k f) -> p k f", k=8)
                wu = wu_[:].rearrange("p (k f) -> p k f", k=8)
                wd = wd_[:].rearrange("p (j d) -> p j d", j=16)
                K.dma(K.POOL, wg, I[f"wg{l}"][ei].rearrange("(k p) f -> p k f", p=128), w=[wg_])
                K.dma(K.POOL, wu, I[f"wu{l}"][ei].rearrange("(k p) f -> p k f", p=128), w=[wu_])
                K.dma(K.POOL, wd, I[f"wd{l}"][ei].rearrange("(j p) d -> p j d", p=128), w=[wd_])
                for (sname, ta, tb, cap) in sets:
                    XIN = F["XINL"] if sname == "L" else F["XINC"]
                    OUT = F["OUTL"] if sname == "L" else F["OUTC"]
                    nst = (cap + 127) // 128
                    gs = gs_ring.next()
                    for st in range(nst):
                        ns = min(128, cap - st * 128)
                        xin = xin_ring.next()
                        K.dma(K.SP, xin[:ns, :], XIN[ei][st * 128:st * 128 + ns, :], r=[("XIN", sname, ei)], w=[xin])
                        K.copy(K.DVE, gs[:ns, st, :], xin[:ns, 1024:ROWW].bitcast(F32), r=[xin], w=[gs])
                        for half in range(2):
                            pp = px.next()
                            ppb = pp[:].bitcast(BF16)
                            for kk in range(4):
                                k = half * 4 + kk
                                K.transpose(ppb[:, kk * 128:kk * 128 + ns], xin[:ns, k * 128:(k + 1) * 128].bitcast(BF16), cst["identb"][:ns, :ns], r=[xin], w=[pp])
                            K.copy(K.ACT if half == 0 else K.DVE, xinT[:, half * 4:(half + 1) * 4, st * 128:st * 128 + ns],
                                   ppb[:, 0:512].rearrange("p (k q) -> p k q", q=128)[:, :, :ns], r=[pp], w=[("xinT", st)])
                    sblocks = [(b0, min(512, cap - b0)) for b0 in range(0, cap, 512)]
                    for j in range(16):
                        for (b0, bn) in sblocks:
                            rk = [("xinT", st) for st in range(b0 // 128, (b0 + bn + 127) // 128)]
                            g_, u_ = pg.next(), pu.next()
                            for k in range(8):
                                K.mm(g_[:, :bn], wg[:, k, j * 128:(j + 1) * 128], xinT[:, k, b0:b0 + bn], start=(k == 0), stop=(k == 7), r=[wg_] + rk, w=[g_])
                            for k in range(8):
                                K.mm(u_[:, :bn], wu[:, k, j * 128:(j + 1) * 128], xinT[:, k, b0:b0 + bn], start=(k == 0), stop=(k == 7), r=[wu_] + rk, w=[u_])
                            sg = sg_ring.next()
                            K.act(sg[:, :bn], g_[:, :bn], AF.Silu, r=[g_], w=[sg])
                            K.tt(K.DVE, hidT[:, j, b0:b0 + bn], u_[:, :bn], sg[:, :bn], ALU.mult, r=[u_, sg], w=[("hidT", j, b0)])
                    for st in range(nst):
                        ns = min(128, cap - st * 128)
                        ob = o_ring.next()
                        for dh in range(2):
                            o_ = po.next()
                            for j in range(16):
                                K.mm(o_[:ns, :], hidT[:, j, st * 128:st * 128 + ns], wd[:, j, dh * 512:(dh + 1) * 512], start=(j == 0), stop=(j == 15),
                                     r=[wd_] + [("hidT", j, (st * 128) // 512 * 512)], w=[o_])
                            K.ts(K.DVE, ob[:ns, dh * 512:(dh + 1) * 512], o_[:ns, :], gs[:ns, st, e:e + 1], ALU.mult, r=[o_, gs], w=[(id(ob), dh)])
                        K.dma(K.SP, OUT[ei][st * 128:st * 128 + ns, :], ob[:ns, :], r=[(id(ob), 0), (id(ob), 1)], w=[("OUT", sname, ei)])
        with K.scope():
            g_ring = Ring(K, "gath", [128, 1024], F32, 4)
            acc_ring = Ring(K, "cacc", [128, 1024], F32, 2)
            x1_ring = Ring(K, "x1t", [128, 8, 128], F32, 2)
            o_ring = Ring(K, "x2t", [128, 8, 128], F32, 2)
            ptr = Ring(K, "pct", [128, 512], F32, 4, psum=True)
            moe_tiles = set()
            for (sname, ta, tb, cap) in sets:
                moe_tiles.update(range(ta, tb))
            for t in range(D.nt):
                r = 1 if t < D.ntc else 0
                x1 = x1_ring.next()
                K.dma(K.SP, x1[:], X1T[:, t * 128:(t + 1) * 128].rearrange("(k p) t -> p k t", p=128), w=[x1])
                xo = o_ring.next()
                if t not in moe_tiles:
                    K.ts(K.DVE, xo[:], x1[:], float(out_scale), ALU.mult, r=[x1], w=[xo])
                else:
                    sname = "C" if t < D.ntc else "L"
                    cap = D.TC // 8 if sname == "C" else D.TL // 8
                    OUT = F["OUTL"] if sname == "L" else F["OUTC"]
                    acc = acc_ring.next()
                    K.memset(K.DVE, acc[:], 0.0, w=[acc])
                    for ei, e in enumerate(exp_ids):
                        g = g_ring.next()
                        K.memset(K.POOL, g[:], 0.0, w=[g])
                        K.dma(K.POOL, lambda q, ei=ei, e=e, t=t, g=g, OUT=OUT, cap=cap: q.indirect_dma_start(
                            out=g[:], out_offset=None, in_=OUT[ei],
                            in_offset=bass.IndirectOffsetOnAxis(ap=POS[:, t, e:e + 1], axis=0),
                            bounds_check=K.bound_reg(cap - 1), oob_is_err=False),
                            None, r=[("OUT", sname, ei)], w=[g])
                        K.tt(K.DVE, acc[:], acc[:], g[:], ALU.add, r=[acc, g], w=[acc])
                    for half in range(2):
                        pp = ptr.next()
                        for kk in range(4):
                            k = half * 4 + kk
                            K.transpose(pp[:, kk * 128:(kk + 1) * 128], acc[:, k * 128:(k + 1) * 128], cst["ident"][:], r=[acc], w=[pp])
                        for kk in range(4):
                            k = half * 4 + kk
                            K.ts(K.DVE, xo[:, k, :], pp[:, kk * 128:(kk + 1) * 128], modT[:, 40 + k, r:r + 1], ALU.mult, r=[pp, modT], w=[xo])
                    K.stt(K.DVE, xo[:], x1[:], float(out_scale), xo[:], ALU.mult, ALU.add, r=[x1, xo], w=[xo])
                K.dma(K.SP, outT[:, t * 128:(t + 1) * 128].rearrange("(k p) t -> p k t", p=128), xo[:], r=[xo], w=[("outT", t)])


PER_S = ("w_core", "cw", "cbias", "dtb", "alog", "dsk", "snw", "rdec", "gnw", "wsso", "wro")
SHARED = ("w_mod", "b_modT", "nmw", "nfw", "wo")
DEPTH_ = 2


def build_final(K, D, I, cst, XT, outT):
    with K.scope():
        fw = K.sb("fw", [128, 8], F32)
        K.dma(K.SP, fw[:], I["fnw"], w=[fw])
        xring = Ring(K, "fxb", [128, 8, 512], F32, 2)
        sq = K.sb("fsq", [128, 8, 512], F32)
        rstd = K.sb("frstd", [128, 512], F32)
        oring = Ring(K, "fo", [128, 8, 512], F32, 2)
        pring = Ring(K, "fpn", [128, 512], F32, 2, psum=True)
        for (s0, n, r) in D.blocks[1:]:
            xb = xring.next()
            K.dma(K.SP, xb[:], XT[:, s0:s0 + n].rearrange("(k p) t -> p k t", p=128), w=[xb])
            K.act(sq[:], xb[:], AF.Square, r=[xb], w=[sq])
            ps = pring.next()
            for k in range(8):
                K.mm(ps[:], cst["ones"][:], sq[:, k, :], start=(k == 0), stop=(k == 7), r=[sq], w=[ps])
            rsqrt_mean(K, rstd[:], ps[:], 1024, r=[ps], w=[rstd])
            ob = oring.next()
            for k in range(8):
                K.stt(K.DVE, ob[:, k, :], xb[:, k, :], fw[:, k:k + 1], rstd[:], ALU.mult, ALU.mult,
                      r=[xb, rstd, fw], w=[(id(ob), k)])
            K.dma(K.ACT, outT[:, s0 - D.TC:s0 - D.TC + n].rearrange("(k p) t -> p k t", p=128), ob[:],
                  r=[(id(ob), k) for k in range(8)], is_output=True)


def full_input_specs(D):
    specs = {"xT": ([1024, D.T], F32), "condT": ([128, 8, 2], F32), "cos": ([D.TL, 64], F32), "sin": ([D.TL, 64], F32),
             "fnw": ([128, 8], F32)}
    for l in range(DEPTH_):
        ms = mixer_input_specs(D, l)
        for base in SHARED:
            specs[f"{base}{l}"] = ms[f"{base}{l}"]
        for s in range(2):
            for base in PER_S:
                specs[f"{base}{l}s{s}"] = ms[f"{base}{l}"]
        specs.update(ffn_input_specs(l, 16))
    return specs


def build_full(D):
    nc = bass.Bass("TRN2", target_bir_lowering=False)
    I = {k: nc.dram_tensor(k, shp, dt, kind="ExternalInput").ap() for k, (shp, dt) in full_input_specs(D).items()}
    outT = nc.dram_tensor("outT", [1024, D.TL], F32, kind="ExternalOutput").ap()
    def dram(name):
        return nc.dram_tensor(name, [1024, D.T], F32, kind="Internal").ap()
    K = KB(nc)
    S = alloc_mixer_scratch(nc, D)
    F = alloc_ffn_scratch(nc, D, 16)
    cst = make_consts(K)
    modT = K.sb("modT", [128, 48, 2], F32)
    XT = I["xT"]
    deltas = [dram("DELTA0"), dram("DELTA1")]
    X1T = dram("X1T")
    xnext = [dram("XN0"), dram("XN1")]
    for l in range(DEPTH_):
        for s in range(2):
            tag = f"{l}s{s}"
            Iv = dict(I)
            for base in SHARED:
                Iv[f"{base}{tag}"] = I[f"{base}{l}"]
            build_mixer_layer(K, nc, D, Iv, tag, S, cst, modT, [XT], deltas[s])
        last = (l == DEPTH_ - 1)
        build_ffn_layer(K, nc, D, I, l, F, cst, modT, [XT] + deltas, X1T, xnext[l], list(range(16)), not last, 1.0)
        XT = xnext[l]
    build_final(K, D, I, cst, XT, outT)
    K.finish()
    return nc, K


def prep_core_inputs(inp, b, D):
    m = prep_common_inputs(inp, b, D)
    m["fnw"] = fm(inp["final_norm_w"], 8)
    for l in range(DEPTH_):
        for s in range(2):
            pm = prep_mixer_inputs(inp, l, s)
            for base in SHARED:
                m[f"{base}{l}"] = pm[f"{base}{l}"]
            for base in PER_S:
                m[f"{base}{l}s{s}"] = pm[f"{base}{l}"]
        m[f"w_router{l}"] = np.ascontiguousarray(inp["w_router"][l], np.float32)
        m[f"wg{l}"] = np.ascontiguousarray(inp["w_gate"][l], np.float32)
        m[f"wu{l}"] = np.ascontiguousarray(inp["w_up"][l], np.float32)
        m[f"wd{l}"] = np.ascontiguousarray(inp["w_down"][l], np.float32)
    return m


def kernel(**inputs):
    inp = {k: np.asarray(v) for k, v in inputs.items()}
    B = inp["x"].shape[0]
    D = Dims(inp["ctx"].shape[1] // 128, inp["x"].shape[1] // 128)
    nc, _ = build_full(D)
    in_maps = [prep_core_inputs(inp, b, D) for b in range(B)]
    res = run_bass_kernel_spmd(nc, in_maps, core_ids=list(range(B)))
    out = np.stack([np.ascontiguousarray(res.results[b]["outT"].T) for b in range(B)], axis=0)
    return out.astype(np.float32)
```
